# Optimizing a Trainium2 kernel written in Bass

```python
import jax
import jax.numpy as jnp
from jax import lax
import numpy as np

D_MODEL = 4096
BATCH = 1
SEQ = 8192
DEPTH = 1

ATT_PATTERNS = ((128, 1), (512, 4), (2048, 16))
N_ATT_GROUPS = 3
ATT_HEADS = 8
ATT_HEAD_DIM = 128
ATT_BLOCK = 64
ROPE_THETA = 10000.0
ATT_GROUP_W = ATT_HEADS * ATT_HEAD_DIM
ATT_W = N_ATT_GROUPS * ATT_GROUP_W

ML_HEADS = 8
ML_QK_DIM = 256
ML_V_DIM = 512
ML_CHUNK = 64
GATE_SOFTCAP = 15.0
ML_QK_W = ML_HEADS * ML_QK_DIM
ML_V_W = ML_HEADS * ML_V_DIM
N_ML_GATES = 4 * ML_HEADS

N_GROUPS = 8
EXPERTS_PER_GROUP = 8
N_EXPERTS = N_GROUPS * EXPERTS_PER_GROUP
TOP_K = 2
D_EXPERT = 768
MOE_BLOCK = 128

IN_SPLIT = (ATT_W, ATT_W, ATT_W, ML_QK_W, ML_QK_W, ML_V_W, ML_V_W, N_ML_GATES, D_MODEL, D_MODEL)
D_IN = sum(IN_SPLIT)
EPS = 1e-6
NEG = -1e30

kernel_name = 'hybrid_dilated_attn_mlstm_hier_moe'


def rms_norm(x, gain):
    xf = x.astype(jnp.float32)
    y = xf * lax.rsqrt(jnp.mean(xf * xf, axis=-1, keepdims=True) + EPS)
    return (y * gain.astype(jnp.float32)).astype(x.dtype)


def rotary(x, pos):
    half = x.shape[-1] // 2
    inv_freq = ROPE_THETA ** (-jnp.arange(half, dtype=jnp.float32) / half)
    ang = pos.astype(jnp.float32)[:, None] * inv_freq[None, :]
    shape = (1, x.shape[1]) + (1,) * (x.ndim - 3) + (half,)
    cos = jnp.cos(ang).reshape(shape)
    sin = jnp.sin(ang).reshape(shape)
    xf = x.astype(jnp.float32)
    x1, x2 = xf[..., :half], xf[..., half:]
    return jnp.concatenate([x1 * cos - x2 * sin, x2 * cos + x1 * sin], axis=-1).astype(x.dtype)


def dilated_window_attention(q, k, v, window, dilation):
    B, S, H, Dh = q.shape
    radius = window // (2 * dilation)
    blk = ATT_BLOCK
    assert radius <= blk
    L = S // dilation
    nb = -(-L // blk)
    Lp = nb * blk

    def to_residue(t):
        t = t.reshape(B, L, dilation, H, Dh).transpose(0, 2, 3, 1, 4)
        return jnp.pad(t, ((0, 0), (0, 0), (0, 0), (0, Lp - L), (0, 0)))

    def windows(t):
        tp = jnp.pad(t, ((0, 0), (0, 0), (0, 0), (blk, blk), (0, 0)))
        tp = tp.reshape(B, dilation, H, nb + 2, blk, Dh)
        return jnp.concatenate([tp[:, :, :, 0:nb], tp[:, :, :, 1:nb + 1], tp[:, :, :, 2:nb + 2]], axis=4)

    qb = to_residue(q).reshape(B, dilation, H, nb, blk, Dh).astype(jnp.float32)
    kw = windows(to_residue(k)).astype(jnp.float32)
    vw = windows(to_residue(v)).astype(jnp.float32)
    scores = jnp.einsum('brhnqd,brhnkd->brhnqk', qb, kw) * (Dh ** -0.5)
    uq = jnp.arange(nb)[:, None, None] * blk + jnp.arange(blk)[None, :, None]
    uk = (jnp.arange(nb)[:, None, None] - 1) * blk + jnp.arange(3 * blk)[None, None, :]
    valid = (jnp.abs(uk - uq) <= radius) & (uk >= 0) & (uk < L)
    scores = jnp.where(valid, scores, NEG)
    lse = jax.nn.logsumexp(scores, axis=-1)
    p = jnp.exp(scores - lse[..., None])
    o = jnp.einsum('brhnqk,brhnkd->brhnqd', p, vw)
    o = o.reshape(B, dilation, H, Lp, Dh)[:, :, :, :L].transpose(0, 3, 1, 2, 4).reshape(B, S, H, Dh)
    lse = lse.reshape(B, dilation, H, Lp)[..., :L].transpose(0, 3, 1, 2).reshape(B, S, H)
    return o, lse


def dilated_attention_mixer(aq, ak, av, q_gain, k_gain):
    B, S, _ = aq.shape
    shp = (B, S, N_ATT_GROUPS, ATT_HEADS, ATT_HEAD_DIM)
    pos = jnp.arange(S)
    q = rotary(rms_norm(aq.reshape(shp), q_gain[:, None, :]), pos)
    k = rotary(rms_norm(ak.reshape(shp), k_gain[:, None, :]), pos)
    v = av.reshape(shp)
    outs, lses = [], []
    for g, (window, dilation) in enumerate(ATT_PATTERNS):
        o, lse = dilated_window_attention(q[:, :, g], k[:, :, g], v[:, :, g], window, dilation)
        outs.append(o)
        lses.append(lse)
    o = jnp.stack(outs)
    w = jax.nn.softmax(jnp.stack(lses), axis=0)
    o = jnp.sum(w[..., None] * o, axis=0)
    return o.reshape(B, S, ATT_GROUP_W).astype(aq.dtype)


def mlstm_chunk_step(carry, xs):
    C, n, m = carry
    q, k, v, ig, lf = xs
    Lc = q.shape[2]
    b = jnp.cumsum(lf, axis=-1)
    log_d = b[..., :, None] - b[..., None, :] + ig[..., None, :]
    tri = jnp.tril(jnp.ones((Lc, Lc), dtype=bool))
    log_d = jnp.where(tri, log_d, NEG)
    m_inter = b + m[..., None]
    m_t = jnp.maximum(m_inter, jnp.max(log_d, axis=-1))
    decay_q = jnp.exp(m_inter - m_t)
    s = jnp.einsum('nhtd,nhsd->nhts', q, k) * jnp.exp(log_d - m_t[..., None])
    num = jnp.einsum('nhts,nhsv->nhtv', s, v) + decay_q[..., None] * jnp.einsum('nhtd,nhdv->nhtv', q, C)
    den = jnp.sum(s, axis=-1) + decay_q * jnp.einsum('nhtd,nhd->nht', q, n)
    h = num / jnp.maximum(jnp.abs(den), jnp.exp(-m_t))[..., None]
    m_last = m_t[..., -1]
    w_s = jnp.exp(b[..., -1:] - b + ig - m_last[..., None])
    decay_c = jnp.exp(b[..., -1] + m - m_last)
    C = decay_c[..., None, None] * C + jnp.einsum('nhs,nhsd,nhsv->nhdv', w_s, k, v)
    n = decay_c[..., None] * n + jnp.einsum('nhs,nhsd->nhd', w_s, k)
    return (C, n, m_last), h


def mlstm_mixer(mq, mk, mv, mo, mg, gate_bias, head_gain):
    B, S, _ = mq.shape
    f32 = jnp.float32
    q = mq.astype(f32).reshape(B, S, ML_HEADS, ML_QK_DIM)
    k = mk.astype(f32).reshape(B, S, ML_HEADS, ML_QK_DIM) * (ML_QK_DIM ** -0.5)
    v = mv.astype(f32).reshape(B, S, ML_HEADS, ML_V_DIM)
    pre = mg.astype(f32).reshape(B, S, 4, ML_HEADS) + gate_bias.astype(f32)
    pre = GATE_SOFTCAP * jnp.tanh(pre / GATE_SOFTCAP)
    i_fw, f_fw, i_bw, f_bw = pre[:, :, 0], pre[:, :, 1], pre[:, :, 2], pre[:, :, 3]

    def rev(t):
        return jnp.flip(t, axis=1)

    qd = jnp.concatenate([q, rev(q)], axis=0)
    kd = jnp.concatenate([k, rev(k)], axis=0)
    vd = jnp.concatenate([v, rev(v)], axis=0)
    igd = jnp.concatenate([i_fw, rev(i_bw)], axis=0)
    lfd = jnp.concatenate([jax.nn.log_sigmoid(f_fw), rev(jax.nn.log_sigmoid(f_bw))], axis=0)
    N2 = 2 * B
    nc = S // ML_CHUNK

    def chunk4(t):
        return t.reshape(N2, nc, ML_CHUNK, ML_HEADS, t.shape[-1]).transpose(1, 0, 3, 2, 4)

    def chunk3(t):
        return t.reshape(N2, nc, ML_CHUNK, ML_HEADS).transpose(1, 0, 3, 2)

    carry0 = (jnp.zeros((N2, ML_HEADS, ML_QK_DIM, ML_V_DIM), f32),
              jnp.zeros((N2, ML_HEADS, ML_QK_DIM), f32),
              jnp.zeros((N2, ML_HEADS), f32))
    _, hs = lax.scan(mlstm_chunk_step, carry0, (chunk4(qd), chunk4(kd), chunk4(vd), chunk3(igd), chunk3(lfd)))
    hs = hs.transpose(1, 0, 3, 2, 4).reshape(N2, S, ML_HEADS, ML_V_DIM)
    h = hs[:B] + rev(hs[B:])
    h = rms_norm(h, head_gain) * jax.nn.sigmoid(mo.astype(f32)).reshape(B, S, ML_HEADS, ML_V_DIM)
    return h.reshape(B, S, ML_V_W).astype(mq.dtype)


def hier_moe(h, w_rg, b_rg, w_re, b_re, w_gate, w_up, w_down):
    N, D = h.shape
    glog = (h @ w_rg + b_rg).astype(jnp.float32)
    gprob = jax.nn.softmax(glog, axis=-1)
    g_sel = jnp.argmax(glog, axis=-1)
    p_g = jnp.take_along_axis(gprob, g_sel[:, None], axis=1)[:, 0]
    elog = (h @ w_re + b_re).astype(jnp.float32).reshape(N, N_GROUPS, EXPERTS_PER_GROUP)
    elog = jnp.take_along_axis(elog, g_sel[:, None, None], axis=1)[:, 0]
    top_v, top_i = lax.top_k(elog, TOP_K)
    weights = p_g[:, None] * jax.nn.softmax(top_v, axis=-1)
    expert_ids = (g_sel[:, None] * EXPERTS_PER_GROUP + top_i).astype(jnp.int32)

    A = N * TOP_K
    blk = MOE_BLOCK
    flat_e = expert_ids.reshape(A)
    flat_tok = jnp.repeat(jnp.arange(N, dtype=jnp.int32), TOP_K)
    flat_w = weights.reshape(A)
    order = jnp.argsort(flat_e)
    se = flat_e[order]
    counts = jnp.zeros((N_EXPERTS,), jnp.int32).at[flat_e].add(1)
    padded = ((counts + blk - 1) // blk) * blk
    pad_end = jnp.cumsum(padded)
    pad_start = pad_end - padded
    start = jnp.cumsum(counts) - counts
    dest = pad_start[se] + jnp.arange(A, dtype=jnp.int32) - start[se]
    P = A + N_EXPERTS * blk
    nblk = P // blk
    slot_tok = jnp.full((P,), N, jnp.int32).at[dest].set(flat_tok[order])
    slot_w = jnp.zeros((P,), jnp.float32).at[dest].set(flat_w[order])
    blk_expert = jnp.minimum(jnp.searchsorted(pad_end, jnp.arange(nblk, dtype=jnp.int32) * blk, side='right'),
                             N_EXPERTS - 1)
    h_pad = jnp.concatenate([h, jnp.zeros((1, D), h.dtype)], axis=0)
    xs = h_pad[slot_tok].reshape(nblk, blk, D)

    def expert_block(args):
        xb, e = args
        return (jax.nn.silu(xb @ w_gate[e]) * (xb @ w_up[e])) @ w_down[e]

    ys = lax.map(expert_block, (xs, blk_expert)).reshape(P, D)
    ys = ys * slot_w[:, None].astype(ys.dtype)
    return jnp.zeros((N + 1, D), ys.dtype).at[slot_tok].add(ys)[:N]


def setup_inputs(seed: int = 0) -> dict:
    key = jax.random.key(seed)
    ks = jax.random.split(key, 20)
    f32 = jnp.float32

    def nrm(k, shape, scale):
        return jax.random.normal(k, shape, f32) * scale

    i_bias = -1.0 + 0.1 * jax.random.normal(ks[5], (DEPTH, 2, ML_HEADS), f32)
    f_bias = 3.0 + 3.0 * jax.random.uniform(ks[6], (DEPTH, 2, ML_HEADS), f32)
    mlstm_gate_bias = jnp.stack([i_bias[:, 0], f_bias[:, 0], i_bias[:, 1], f_bias[:, 1]], axis=1)
    return {
        'x': nrm(ks[0], (BATCH, SEQ, D_MODEL), 1.0),
        'norm1_gain': 1.0 + nrm(ks[1], (DEPTH, D_MODEL), 0.01),
        'w_in': nrm(ks[2], (DEPTH, D_MODEL, D_IN), D_MODEL ** -0.5),
        'attn_q_norm_gain': 1.0 + nrm(ks[3], (DEPTH, N_ATT_GROUPS, ATT_HEAD_DIM), 0.01),
        'attn_k_norm_gain': 1.0 + nrm(ks[4], (DEPTH, N_ATT_GROUPS, ATT_HEAD_DIM), 0.01),
        'mlstm_gate_bias': mlstm_gate_bias,
        'mlstm_head_norm_gain': 1.0 + nrm(ks[7], (DEPTH, ML_HEADS, ML_V_DIM), 0.01),
        'branch_gate_bias': nrm(ks[8], (DEPTH, 2, D_MODEL), 0.1),
        'w_attn_branch': nrm(ks[9], (DEPTH, ATT_GROUP_W, D_MODEL), ATT_GROUP_W ** -0.5),
        'w_mlstm_branch': nrm(ks[10], (DEPTH, ML_V_W, D_MODEL), ML_V_W ** -0.5),
        'w_out': nrm(ks[11], (DEPTH, D_MODEL, D_MODEL), D_MODEL ** -0.5),
        'norm2_gain': 1.0 + nrm(ks[12], (DEPTH, D_MODEL), 0.01),
        'w_router_group': nrm(ks[13], (DEPTH, D_MODEL, N_GROUPS), D_MODEL ** -0.5),
        'b_router_group': nrm(ks[14], (DEPTH, N_GROUPS), 0.01),
        'w_router_expert': nrm(ks[15], (DEPTH, D_MODEL, N_EXPERTS), D_MODEL ** -0.5),
        'b_router_expert': nrm(ks[16], (DEPTH, N_EXPERTS), 0.01),
        'w_expert_gate': nrm(ks[17], (DEPTH, N_EXPERTS, D_MODEL, D_EXPERT), D_MODEL ** -0.5),
        'w_expert_up': nrm(ks[18], (DEPTH, N_EXPERTS, D_MODEL, D_EXPERT), D_MODEL ** -0.5),
        'w_expert_down': nrm(ks[19], (DEPTH, N_EXPERTS, D_EXPERT, D_MODEL), D_EXPERT ** -0.5),
    }


def reference(x, norm1_gain, w_in, attn_q_norm_gain, attn_k_norm_gain, mlstm_gate_bias,
              mlstm_head_norm_gain, branch_gate_bias, w_attn_branch, w_mlstm_branch, w_out,
              norm2_gain, w_router_group, b_router_group, w_router_expert, b_router_expert,
              w_expert_gate, w_expert_up, w_expert_down):
    B, S, D = x.shape
    offs = []
    acc = 0
    for width in IN_SPLIT[:-1]:
        acc += width
        offs.append(acc)
    for l in range(DEPTH):
        h = rms_norm(x, norm1_gain[l])
        proj = h @ w_in[l]
        aq, ak, av, mq, mk, mv, mo, mg, gate_a, gate_m = jnp.split(proj, offs, axis=-1)
        att = dilated_attention_mixer(aq, ak, av, attn_q_norm_gain[l], attn_k_norm_gain[l])
        mem = mlstm_mixer(mq, mk, mv, mo, mg, mlstm_gate_bias[l], mlstm_head_norm_gain[l])
        g_a = jax.nn.sigmoid(gate_a + branch_gate_bias[l, 0])
        g_m = jax.nn.sigmoid(gate_m + branch_gate_bias[l, 1])
        merged = g_a * (att @ w_attn_branch[l]) + g_m * (mem @ w_mlstm_branch[l])
        x = x + merged @ w_out[l]
        h2 = rms_norm(x, norm2_gain[l]).reshape(B * S, D)
        moe = hier_moe(h2, w_router_group[l], b_router_group[l], w_router_expert[l], b_router_expert[l],
                       w_expert_gate[l], w_expert_up[l], w_expert_down[l])
        x = x + moe.reshape(B, S, D)
    return x
```

```python
import contextlib
import numpy as np
import concourse.bass as bass
import concourse.mybir as mybir
from concourse.bass_utils import run_bass_kernel_spmd

F32 = mybir.dt.float32
BF16 = mybir.dt.bfloat16
AF = mybir.ActivationFunctionType
ALU = mybir.AluOpType
AX = mybir.AxisListType

ENGS = ("sync", "scalar", "gpsimd", "vector", "tensor")


class Buf:
    __slots__ = ("w", "r", "excl")

    def __init__(self, excl=False):
        self.w = None
        self.r = {}
        self.excl = excl


class Sched:
    EPOCH = 12000
    NPOOL = 40

    def __init__(self, nc, stack):
        self.nc = nc
        self.stack = stack
        self.q = {e: [] for e in ENGS}
        self.cur = {}
        self.seen = {e: {} for e in ENGS}
        self.pool = []
        self.pool_i = 0
        self.nsem = 0

    def _newsem(self):
        self.nsem += 1
        return self.stack.enter_context(self.nc.semaphore("s%d" % self.nsem))

    def emit(self, eng, fn, reads=(), writes=(), dma=False):
        deps = {}

        def add(t):
            if t is None:
                return
            k = id(t[0])
            if k not in deps or deps[k][1] < t[1]:
                deps[k] = t

        reads = list(reads)
        writes = list(writes)
        for b in list(reads):
            if b.excl:
                reads.remove(b)
                writes.append(b)
        for b in reads:
            add(b.w)
        for b in writes:
            add(b.w)
            for t in b.r.values():
                add(t)
        if dma:
            if len(self.pool) < self.NPOOL:
                self.pool.append([self._newsem(), 0])
                slot = self.pool[-1]
            else:
                slot = self.pool[self.pool_i % self.NPOOL]
                self.pool_i += 1
            if slot[1] > 0:
                add((slot[0], 16 * slot[1]))
            slot[1] += 1
            tok = (slot[0], 16 * slot[1])
            inc = 16
        else:
            c = self.cur.get(eng)
            if c is None or c[1] >= self.EPOCH:
                c = [self._newsem(), 0]
                self.cur[eng] = c
            c[1] += 1
            tok = (c[0], c[1])
            inc = 1
        waits = []
        seen = self.seen[eng]
        own = self.cur.get(eng)
        for k, t in deps.items():
            if eng == "tensor" and own is not None and t[0] is own[0]:
                continue
            if seen.get(k, 0) < t[1]:
                seen[k] = t[1]
                waits.append(t)
        self.q[eng].append((waits, fn, tok, inc))
        ktok = id(tok[0])
        for b in reads:
            if ktok not in b.r or b.r[ktok][1] < tok[1]:
                b.r[ktok] = tok
        for b in writes:
            b.w = tok
            b.r = {}
        return tok

    def run_phase(self):
        allw = []
        for s, n in self.pool:
            if n > 0:
                allw.append((s, 16 * n))
        for e, c in self.cur.items():
            allw.append((c[0], c[1]))
        for e in ENGS:
            waits = []
            for t in allw:
                k = id(t[0])
                if self.seen[e].get(k, 0) < t[1]:
                    self.seen[e][k] = t[1]
                    waits.append(t)
            self.q[e].append((waits, None, None, 0))
        nc = self.nc
        q = self.q
        self.q = {e: [] for e in ENGS}
        with nc.Block() as block:
            def mk(name):
                def body(e):
                    for waits, fn, tok, inc in q[name]:
                        for s, v in waits:
                            e.wait_ge(s, v)
                        if fn is not None:
                            ins = fn(e)
                            ins.then_inc(tok[0], inc)
                return body
            block.sync(mk("sync"))
            block.scalar(mk("scalar"))
            block.gpsimd(mk("gpsimd"))
            block.vector(mk("vector"))
            block.tensor(mk("tensor"))


SEQ = 8192
CUT = 99
DM = 4096
NT = SEQ // 128
EPS = 1e-6
ATT = ((0, 1), (1, 4), (2, 16))


_UID = [0]


def mkalloc(nc, st):
    def sb(name, shape, dt):
        _UID[0] += 1
        return st.enter_context(nc.sbuf_tensor("%s_%d" % (name, _UID[0]), shape, dt))

    def ps(name, shape, dt):
        _UID[0] += 1
        return st.enter_context(nc.psum_tensor("%s_%d" % (name, _UID[0]), shape, dt))
    return sb, ps


def make_ident(S, sb, n=128):
    identf = sb("identf", [128, 128], F32)
    ident = sb("ident", [128, 128], BF16)
    bid = Buf()
    S.emit("gpsimd", lambda e: e.memset(identf[:], 1.0), writes=[bid])
    S.emit("gpsimd", lambda e: e.affine_select(out=identf[:], in_=identf[:], pattern=[[-1, 128]], compare_op=ALU.is_equal,
                                               fill=0.0, base=0, channel_multiplier=1), reads=[bid], writes=[bid])
    S.emit("gpsimd", lambda e: e.tensor_copy(out=ident[:], in_=identf[:]), reads=[bid], writes=[bid])
    return ident, identf, bid


def x_pass(nc, S, st, x, w_dram, ncols, g1t, per_tile, ntiles=NT):
    sb, ps = mkalloc(nc, st)
    W = sb("W", [128, 32, ncols], BF16); bW = Buf()
    wst = [sb("wst%d" % i, [128, ncols], F32) for i in range(2)]
    bwst = [Buf(), Buf()]
    g1 = sb("g1", [128, 32], F32); bg1 = Buf()
    epsb = sb("epsb", [128, 1], F32); beps = Buf()
    ident, identf, bid = make_ident(S, sb)
    S.emit("sync", lambda e: e.dma_start(out=g1[:], in_=g1t), writes=[bg1], dma=True)
    S.emit("vector", lambda e: e.memset(epsb[:], EPS), writes=[beps])
    for c in range(32):
        S.emit("sync", lambda e, c=c: e.dma_start(out=wst[c % 2][:], in_=w_dram[c * 128:(c + 1) * 128, :]), writes=[bwst[c % 2]], dma=True)
        S.emit("vector", lambda e, c=c: e.tensor_scalar(out=W[:, c, :], in0=wst[c % 2][:], scalar1=g1[:, c:c + 1], scalar2=None, op0=ALU.mult),
               reads=[bwst[c % 2], bg1], writes=[bW])
    xb = [sb("xb%d" % i, [128, DM], F32) for i in range(2)]
    bxb = [Buf(), Buf()]
    sq = sb("sq", [128, DM], BF16); bsq = Buf()
    ss = sb("ss", [128, 1], F32); bss = Buf()
    rstd = sb("rstd", [128, 1], F32); brstd = Buf()
    xs = sb("xs", [128, DM], BF16); bxs = Buf()
    hT = sb("hT", [128, 32, 128], BF16); bhT = Buf()
    pT = [ps("pT%d" % i, [128, 8, 128], BF16) for i in range(2)]
    bpT = [Buf(True), Buf(True)]

    def load(t):
        S.emit("sync", lambda e: e.dma_start(out=xb[t % 2][:], in_=x[t * 128:(t + 1) * 128, :]), writes=[bxb[t % 2]], dma=True)

    load(0)
    for t in range(ntiles):
        if t + 1 < ntiles:
            load(t + 1)
        xt = xb[t % 2]; bx = bxb[t % 2]
        S.emit("scalar", lambda e, xt=xt: e.activation(out=sq[:], in_=xt[:], func=AF.Square), reads=[bx], writes=[bsq])
        S.emit("vector", lambda e: e.reduce_sum(out=ss[:], in_=sq[:], axis=AX.X), reads=[bsq], writes=[bss])
        S.emit("scalar", lambda e: e.activation(out=rstd[:], in_=ss[:], func=AF.Sqrt, scale=1.0 / DM, bias=epsb[:]), reads=[bss, beps], writes=[brstd])
        S.emit("vector", lambda e: e.reciprocal(out=rstd[:], in_=rstd[:]), reads=[brstd], writes=[brstd])
        S.emit("vector", lambda e, xt=xt: e.tensor_scalar(out=xs[:], in0=xt[:], scalar1=rstd[:, 0:1], scalar2=None, op0=ALU.mult),
               reads=[bx, brstd], writes=[bxs])
        for c8 in range(4):
            p = pT[c8 % 2]; bp = bpT[c8 % 2]
            for cc in range(8):
                c = c8 * 8 + cc
                S.emit("tensor", lambda e, p=p, cc=cc, c=c: e.transpose(out=p[:, cc, :], in_=xs[:, c * 128:(c + 1) * 128], identity=ident[:]),
                       reads=[bxs, bid], writes=[bp])
            if c8 % 2 == 0:
                S.emit("scalar", lambda e, p=p, c8=c8: e.activation(out=hT[:, c8 * 8:(c8 + 1) * 8, :], in_=p[:], func=AF.Copy), reads=[bp], writes=[bhT])
            else:
                S.emit("vector", lambda e, p=p, c8=c8: e.tensor_copy(out=hT[:, c8 * 8:(c8 + 1) * 8, :], in_=p[:]), reads=[bp], writes=[bhT])
        per_tile(t, hT, bhT, W, bW)


def phase_att_proj(nc, S, x, w_att, g1t, gqk, cs, qkv_s, ntiles=NT):
    with contextlib.ExitStack() as st:
        sb, ps = mkalloc(nc, st)
        gains = sb("gains", [128, 768], F32); bgains = Buf()
        S.emit("sync", lambda e: e.dma_start(out=gains[:], in_=gqk), writes=[bgains], dma=True)
        eps2 = sb("eps2", [128, 1], F32); beps2 = Buf()
        S.emit("vector", lambda e: e.memset(eps2[:], EPS), writes=[beps2])
        pqb = [ps("pq%d" % i, [128, 512], F32) for i in range(3)]
        pq = [p[:, 0:384] for p in pqb]
        bpq = [Buf(True) for _ in range(3)]
        qkf = sb("qkf", [128, 768], F32); bqkf = Buf()
        tmp = sb("tmp", [128, 768], F32); btmp = Buf()
        ss6 = sb("ss6", [128, 6], F32); bss6 = Buf()
        r6 = sb("r6", [128, 6], F32); br6 = Buf()
        qn = sb("qn", [128, 768], F32); bqn = Buf()
        t1 = sb("t1", [128, 6, 64], F32); bt1 = Buf()
        t2 = sb("t2", [128, 6, 64], F32); bt2 = Buf()
        qr = [sb("qr%d" % i, [128, 768], BF16) for i in range(2)]
        bqr = [Buf(), Buf()]
        vb = [sb("vb%d" % i, [128, 384], BF16) for i in range(2)]
        bvb = [Buf(), Buf()]
        cst = [sb("cst%d" % i, [128, 128], F32) for i in range(2)]
        bcst = [Buf(), Buf()]

        def per_tile(t, hT, bhT, W, bW):
            S.emit("sync", lambda e: e.dma_start(out=cst[t % 2][:], in_=cs[t * 128:(t + 1) * 128, :]), writes=[bcst[t % 2]], dma=True)
            for kind in range(3):
                for c in range(32):
                    S.emit("tensor", lambda e, kind=kind, c=c: e.matmul(pq[kind], lhsT=hT[:, c, :], rhs=W[:, c, kind * 384:(kind + 1) * 384],
                                                                        start=(c == 0), stop=(c == 31)),
                           reads=[bhT, bW], writes=[bpq[kind]])
            if CUT < 1:
                return
            S.emit("scalar", lambda e: e.activation(out=qkf[:, 0:384], in_=pq[0], func=AF.Copy), reads=[bpq[0]], writes=[bqkf])
            S.emit("scalar", lambda e: e.activation(out=qkf[:, 384:768], in_=pq[1], func=AF.Copy), reads=[bpq[1]], writes=[bqkf])
            v_ = vb[t % 2]; bv_ = bvb[t % 2]
            S.emit("scalar", lambda e: e.activation(out=v_[:], in_=pq[2], func=AF.Copy), reads=[bpq[2]], writes=[bv_])
            if CUT < 2:
                return
            S.emit("gpsimd", lambda e: e.tensor_tensor(out=tmp[:], in0=qkf[:], in1=qkf[:], op=ALU.mult), reads=[bqkf], writes=[btmp])
            S.emit("vector", lambda e: e.reduce_sum(out=ss6[:], in_=tmp[:].rearrange("p (g c) -> p g c", g=6), axis=AX.X), reads=[btmp], writes=[bss6])
            S.emit("scalar", lambda e: e.activation(out=r6[:], in_=ss6[:], func=AF.Sqrt, scale=1.0 / 128, bias=eps2[:]), reads=[bss6, beps2], writes=[br6])
            S.emit("vector", lambda e: e.reciprocal(out=r6[:], in_=r6[:]), reads=[br6], writes=[br6])
            if CUT < 3:
                return
            S.emit("gpsimd", lambda e: e.tensor_tensor(out=qn[:].rearrange("p (g c) -> p g c", g=6), in0=qkf[:].rearrange("p (g c) -> p g c", g=6),
                                                       in1=r6[:].unsqueeze(2).to_broadcast([128, 6, 128]), op=ALU.mult),
                   reads=[bqkf, br6], writes=[bqn])
            S.emit("gpsimd", lambda e: e.tensor_tensor(out=qn[:], in0=qn[:], in1=gains[:], op=ALU.mult), reads=[bqn, bgains], writes=[bqn])
            if CUT < 4:
                return
            q_ = qr[t % 2]; bq_ = bqr[t % 2]
            c_ = cst[t % 2]; bc_ = bcst[t % 2]
            qv = qn[:].rearrange("p (g h c) -> p g h c", g=6, h=2)
            ov = q_[:].rearrange("p (g h c) -> p g h c", g=6, h=2)
            cosb = c_[:, 0:64].unsqueeze(1).to_broadcast([128, 6, 64])
            sinb = c_[:, 64:128].unsqueeze(1).to_broadcast([128, 6, 64])
            S.emit("vector", lambda e: e.tensor_tensor(out=t1[:], in0=qv[:, :, 0, :], in1=cosb, op=ALU.mult), reads=[bqn, bc_], writes=[bt1])
            S.emit("vector", lambda e: e.tensor_tensor(out=t2[:], in0=qv[:, :, 1, :], in1=sinb, op=ALU.mult), reads=[bqn, bc_], writes=[bt2])
            S.emit("vector", lambda e: e.tensor_tensor(out=ov[:, :, 0, :], in0=t1[:], in1=t2[:], op=ALU.subtract), reads=[bt1, bt2], writes=[bq_])
            S.emit("vector", lambda e: e.tensor_tensor(out=t1[:], in0=qv[:, :, 1, :], in1=cosb, op=ALU.mult), reads=[bqn, bc_], writes=[bt1])
            S.emit("vector", lambda e: e.tensor_tensor(out=t2[:], in0=qv[:, :, 0, :], in1=sinb, op=ALU.mult), reads=[bqn, bc_], writes=[bt2])
            S.emit("vector", lambda e: e.tensor_tensor(out=ov[:, :, 1, :], in0=t1[:], in1=t2[:], op=ALU.add), reads=[bt1, bt2], writes=[bq_])
            if CUT < 5:
                return
            for kind in range(2):
                S.emit("sync", lambda e, kind=kind: e.dma_start(out=qkv_s[kind, :, t * 128:(t + 1) * 128, :].rearrange("g p c -> p g c"),
                                                                in_=q_[:, kind * 384:(kind + 1) * 384].rearrange("p (g c) -> p g c", g=3)),
                       reads=[bq_], dma=True)
            S.emit("sync", lambda e: e.dma_start(out=qkv_s[2, :, t * 128:(t + 1) * 128, :].rearrange("g p c -> p g c"),
                                                 in_=v_[:].rearrange("p (g c) -> p g c", g=3)), reads=[bv_], dma=True)

        x_pass(nc, S, st, x, w_att, 1152, g1t, per_tile, ntiles)
        S.run_phase()


def phase_att_core(nc, S, qkv_s, masks, acc_s, att_out):
    scale = 128 ** -0.5
    with contextlib.ExitStack() as st:
        sb, ps = mkalloc(nc, st)
        ident, identf, bid = make_ident(S, sb)
        mk = sb("mk", [128, 3, 128], BF16); bmk = Buf()
        S.emit("gpsimd", lambda e: e.dma_start(out=mk[:], in_=masks), writes=[bmk], dma=True)
        TM = 64
        Qs = [sb("Qs0", [128, TM, 128], BF16)] * 2
        Ks = [sb("Ks0", [128, TM, 128], BF16)] * 2
        Vs = [sb("Vs%d" % i, [128, TM, 132], BF16) for i in range(2)]
        bQs = [Buf()] * 2; bKs = [Buf()] * 2; bVs = [Buf(), Buf()]
        for i in range(2):
            S.emit("gpsimd", lambda e, i=i: e.memset(Vs[i][:, :, 128:132], 1.0), writes=[bVs[i]])
        QT = sb("QT", [128, TM * 128], BF16); bQT = Buf()
        KT = sb("KT", [128, TM * 128], BF16); bKT = Buf()
        pT = [ps("pTa%d" % i, [128, 8, 128], BF16) for i in range(2)]
        bpT = [Buf(True), Buf(True)]
        pS = [ps("pS%d" % i, [128, 4, 128], F32) for i in range(2)]
        bpS = [Buf(True), Buf(True)]
        pO = [ps("pO%d" % i, [128, 512], F32) for i in range(2)]
        bpO = [Buf(True), Buf(True)]
        PT = [sb("PT%d" % i, [128, 3, 128], BF16) for i in range(2)]
        bPT = [Buf(), Buf()]
        Os = [sb("Os%d" % i, [128, TM, 132], F32) for i in range(2)]
        bOs = [Buf(), Buf()]
        bacc = [Buf() for _ in range(3)]
        units = []
        for g, dil in ATT:
            for r in range(dil):
                units.append((g, dil, r))

        def loadqk(u):
            g, dil, r = units[u]
            T = SEQ // dil // 128
            b = u % 2
            for kind, dst, bd in ((0, Qs[b], bQs[b]), (1, Ks[b], bKs[b])):
                src = qkv_s[kind, g].rearrange("(i p d) c -> d p i c", p=128, d=dil)[r]
                S.emit("sync", lambda e, dst=dst, src=src, T=T: e.dma_start(out=dst[:, 0:T, :], in_=src), writes=[bd], dma=True)

        def load(u):
            g, dil, r = units[u]
            T = SEQ // dil // 128
            b = u % 2
            src = qkv_s[2, g].rearrange("(i p d) c -> d p i c", p=128, d=dil)[r]
            S.emit("sync", lambda e, src=src, T=T, b=b: e.dma_start(out=Vs[b][:, 0:T, 0:128], in_=src), writes=[bVs[b]], dma=True)

        load(0)
        loadqk(0)
        cnt = 0
        for u, (g, dil, r) in enumerate(units):
            T = SEQ // dil // 128
            b = u % 2
            if u + 1 < len(units):
                load(u + 1)
            k8 = 0
            for src, bsrc, dstT, bdst in ((Qs[b], bQs[b], QT, bQT), (Ks[b], bKs[b], KT, bKT)):
                for i0 in range(0, T, 8):
                    n = min(8, T - i0)
                    p = pT[k8 % 2]; bp = bpT[k8 % 2]
                    for ii in range(n):
                        S.emit("tensor", lambda e, p=p, ii=ii, src=src, i0=i0: e.transpose(out=p[:, ii, :], in_=src[:, i0 + ii, :], identity=ident[:]),
                               reads=[bsrc, bid], writes=[bp])
                    dv = dstT[:, i0 * 128:(i0 + n) * 128].rearrange("p (a c) -> p a c", a=n)
                    if k8 % 2 == 0:
                        S.emit("vector", lambda e, p=p, dv=dv, n=n: e.tensor_copy(out=dv, in_=p[:, 0:n, :]), reads=[bp], writes=[bdst])
                    else:
                        S.emit("scalar", lambda e, p=p, dv=dv, n=n: e.activation(out=dv, in_=p[:, 0:n, :], func=AF.Copy), reads=[bp], writes=[bdst])
                    k8 += 1
            if u + 1 < len(units):
                loadqk(u + 1)
            for i in range(T):
                nbs = [n for n in (-1, 0, 1) if 0 <= i + n < T]
                n0, n1 = nbs[0] + 1, nbs[-1] + 2
                s_ = pS[cnt % 2]; bs_ = bpS[cnt % 2]
                for n in nbs:
                    S.emit("tensor", lambda e, s_=s_, n=n, i=i: e.matmul(s_[:, n + 1, :], lhsT=KT[:, (i + n) * 128:(i + n + 1) * 128],
                                                                         rhs=QT[:, i * 128:(i + 1) * 128], start=True, stop=False),
                           reads=[bKT, bQT], writes=[bs_])
                    S.emit("tensor", lambda e, s_=s_, n=n: e.matmul(s_[:, n + 1, :], lhsT=ident[:], rhs=mk[:, n + 1, :], start=False, stop=True),
                           reads=[bid, bmk], writes=[bs_])
                p_ = PT[cnt % 2]; bp_ = bPT[cnt % 2]
                S.emit("scalar", lambda e, s_=s_, p_=p_, n0=n0, n1=n1: e.activation(out=p_[:, n0:n1, :], in_=s_[:, n0:n1, :], func=AF.Exp, scale=scale),
                       reads=[bs_], writes=[bp_])
                o_ = pO[cnt % 2]; bo_ = bpO[cnt % 2]
                for n in nbs:
                    S.emit("tensor", lambda e, o_=o_, p_=p_, n=n, i=i, b=b, st_=(n == nbs[0]), sp_=(n == nbs[-1]): e.matmul(
                        o_[:, 0:129], lhsT=p_[:, n + 1, :], rhs=Vs[b][:, i + n, 0:129], start=st_, stop=sp_),
                           reads=[bp_, bVs[b]], writes=[bo_])
                S.emit("vector", lambda e, o_=o_, i=i, b=b: e.tensor_copy(out=Os[b][:, i, 0:129], in_=o_[:, 0:129]), reads=[bo_], writes=[bOs[b]])
                cnt += 1
            dst = acc_s[g].rearrange("(i p d) c -> d p i c", p=128, d=dil)[r]
            S.emit("scalar", lambda e, dst=dst, T=T, b=b: e.dma_start(out=dst, in_=Os[b][:, 0:T, :]), reads=[bOs[b]], writes=[bacc[g]], dma=True)
        A = [[sb("A%d_%d" % (i, g), [128, 132], F32) for g in range(3)] for i in range(2)]
        bA = [[Buf() for g in range(3)] for i in range(2)]
        rd = sb("rd", [128, 1], F32); brd = Buf()
        ao = [sb("ao%d" % i, [128, 128], F32) for i in range(2)]
        bao = [Buf(), Buf()]
        for t in range(NT):
            b = t % 2
            for g in range(3):
                S.emit("sync", lambda e, g=g, b=b, t=t: e.dma_start(out=A[b][g][:], in_=acc_s[g, t * 128:(t + 1) * 128, :]),
                       reads=[bacc[g]], writes=[bA[b][g]], dma=True)
            S.emit("vector", lambda e, b=b: e.tensor_tensor(out=A[b][0][:], in0=A[b][0][:], in1=A[b][1][:], op=ALU.add),
                   reads=[bA[b][1]], writes=[bA[b][0]])
            S.emit("vector", lambda e, b=b: e.tensor_tensor(out=A[b][0][:], in0=A[b][0][:], in1=A[b][2][:], op=ALU.add),
                   reads=[bA[b][2]], writes=[bA[b][0]])
            S.emit("vector", lambda e, b=b: e.reciprocal(out=rd[:], in_=A[b][0][:, 128:129]), reads=[bA[b][0]], writes=[brd])
            S.emit("vector", lambda e, b=b: e.tensor_scalar(out=ao[b][:], in0=A[b][0][:, 0:128], scalar1=rd[:, 0:1], scalar2=None, op0=ALU.mult),
                   reads=[bA[b][0], brd], writes=[bao[b]])
            S.emit("sync", lambda e, b=b, t=t: e.dma_start(out=att_out[t * 128:(t + 1) * 128, :], in_=ao[b][:]), reads=[bao[b]], dma=True)
        S.run_phase()


def host_consts():
    half = 64
    inv_freq = (10000.0 ** (-np.arange(half, dtype=np.float32) / half)).astype(np.float32)
    ang = np.arange(SEQ, dtype=np.float32)[:, None] * inv_freq[None, :]
    cs = np.concatenate([np.cos(ang), np.sin(ang)], axis=1).astype(np.float32)
    k = np.arange(128)[:, None]
    q = np.arange(128)[None, :]
    masks = np.zeros((128, 3, 128), np.float32)
    for n in (-1, 0, 1):
        valid = np.abs(n * 128 + k - q) <= 64
        masks[:, n + 1, :] = np.where(valid, 0.0, -30000.0)
    return cs, masks


NCH = SEQ // 64
CAP = 15.0


def phase_ml_proj(nc, S, x, w_ml, g1t, gbias, mq_s, mk_s, mv_s, mo_s, mg_s, ntiles=NT):
    with contextlib.ExitStack() as st:
        sb, ps = mkalloc(nc, st)
        gb = sb("gb", [128, 4], F32); bgb = Buf()
        S.emit("sync", lambda e: e.dma_start(out=gb[:], in_=gbias), writes=[bgb], dma=True)
        one = sb("one", [128, 1], F32); bone = Buf()
        S.emit("vector", lambda e: e.memset(one[:], 1.0), writes=[bone])
        pp = [ps("pp%d" % i, [128, 512], F32) for i in range(4)]
        bpp = [Buf(True) for _ in range(4)]
        qk = [sb("qk%d" % i, [128, 512], BF16) for i in range(2)]; bqk = [Buf(), Buf()]
        vv = [sb("vv%d" % i, [128, 512], BF16) for i in range(2)]; bvv = [Buf(), Buf()]
        oo = [sb("oo%d" % i, [128, 512], F32) for i in range(2)]; boo = [Buf(), Buf()]
        gg = [sb("gg%d" % i, [128, 4], F32) for i in range(2)]; bgg = [Buf(), Buf()]
        gt = sb("gt", [128, 4], F32); bgt = Buf()
        ge = sb("ge", [128, 2], F32); bge = Buf()

        def per_tile(t, hT, bhT, W, bW):
            b = t % 2
            segs = ((0, 512), (512, 1024), (1024, 1536), (1536, 1540))
            for si, (a0, a1) in enumerate(segs):
                for c in range(32):
                    S.emit("tensor", lambda e, si=si, a0=a0, a1=a1, c=c: e.matmul(pp[si][:, 0:a1 - a0], lhsT=hT[:, c, :], rhs=W[:, c, a0:a1],
                                                                                 start=(c == 0), stop=(c == 31)),
                           reads=[bhT, bW], writes=[bpp[si]])
            S.emit("scalar", lambda e, b=b: e.activation(out=qk[b][:, 0:256], in_=pp[0][:, 0:256], func=AF.Copy), reads=[bpp[0]], writes=[bqk[b]])
            S.emit("scalar", lambda e, b=b: e.mul(out=qk[b][:, 256:512], in_=pp[0][:, 256:512], mul=1.0 / 16), reads=[bpp[0]], writes=[bqk[b]])
            S.emit("vector", lambda e, b=b: e.tensor_copy(out=vv[b][:], in_=pp[1][:]), reads=[bpp[1]], writes=[bvv[b]])
            S.emit("scalar", lambda e, b=b: e.activation(out=oo[b][:], in_=pp[2][:], func=AF.Sigmoid), reads=[bpp[2]], writes=[boo[b]])
            S.emit("vector", lambda e: e.tensor_tensor(out=gt[:], in0=pp[3][:, 0:4], in1=gb[:], op=ALU.add), reads=[bpp[3], bgb], writes=[bgt])
            S.emit("scalar", lambda e: e.activation(out=gt[:], in_=gt[:], func=AF.Tanh, scale=1.0 / CAP), reads=[bgt], writes=[bgt])
            S.emit("vector", lambda e, b=b: e.tensor_scalar(out=gg[b][:], in0=gt[:], scalar1=CAP, scalar2=None, op0=ALU.mult), reads=[bgt], writes=[bgg[b]])
            gv = gg[b][:].rearrange("p (d two) -> p d two", two=2)
            S.emit("scalar", lambda e, gv=gv: e.activation(out=ge[:], in_=gv[:, :, 1], func=AF.Exp, scale=-1.0), reads=[bgg[b]], writes=[bge])
            S.emit("scalar", lambda e: e.activation(out=ge[:], in_=ge[:], func=AF.Ln, bias=one[:], scale=1.0), reads=[bge, bone], writes=[bge])
            S.emit("vector", lambda e, gv=gv, b=b: e.tensor_scalar(out=gv[:, :, 1], in0=ge[:], scalar1=-1.0, scalar2=None, op0=ALU.mult), reads=[bge], writes=[bgg[b]])
            r0, r1 = t * 128, (t + 1) * 128
            S.emit("sync", lambda e, b=b: e.dma_start(out=mq_s[r0:r1, :], in_=qk[b][:, 0:256]), reads=[bqk[b]], dma=True)
            S.emit("sync", lambda e, b=b: e.dma_start(out=mk_s[r0:r1, :], in_=qk[b][:, 256:512]), reads=[bqk[b]], dma=True)
            S.emit("sync", lambda e, b=b: e.dma_start(out=mv_s[r0:r1, :], in_=vv[b][:]), reads=[bvv[b]], dma=True)
            S.emit("sync", lambda e, b=b: e.dma_start(out=mo_s[r0:r1, :], in_=oo[b][:]), reads=[boo[b]], dma=True)
            S.emit("sync", lambda e, b=b: e.dma_start(out=mg_s[r0:r1, :], in_=gg[b][:]), reads=[bgg[b]], dma=True)

        x_pass(nc, S, st, x, w_ml, 1540, g1t, per_tile, ntiles)
        S.run_phase()


def phase_ml_scan(nc, S, mq_s, mk_s, mv_s, mg_s, tri_d, h_s, nchunks=NCH):
    SC = 8
    with contextlib.ExitStack() as st:
        sb, ps = mkalloc(nc, st)
        ident, identf, bid = make_ident(S, sb)
        tri = sb("tri", [64, 2, 2, 64], F32); btri = Buf()
        S.emit("sync", lambda e: e.dma_start(out=tri[:], in_=tri_d), writes=[btri], dma=True)
        ones_f = sb("ones_f", [64, 128], F32); bof = Buf()
        S.emit("vector", lambda e: e.memset(ones_f[:], 1.0), writes=[bof])
        ones_b = sb("ones_b", [64, 1], BF16); bob = Buf()
        S.emit("vector", lambda e: e.memset(ones_b[:], 1.0), writes=[bob])
        qs = [sb("qs%d" % i, [64, SC, 256], BF16) for i in range(2)]; bqs = [Buf(), Buf()]
        ks = [sb("ks%d" % i, [64, SC, 256], BF16) for i in range(2)]; bks = [Buf(), Buf()]
        vs = [sb("vs%d" % i, [64, SC, 512], BF16) for i in range(2)]; bvs = [Buf(), Buf()]
        gs = [sb("gs%d" % i, [64, SC, 4], F32) for i in range(2)]; bgs = [Buf(), Buf()]
        hs = [sb("hs%d" % i, [64, SC, 512], F32) for i in range(2)]; bhs = [Buf(), Buf()]
        C = sb("C", [128, 2, 512], F32); bC = Buf()
        Cb = sb("Cb", [128, 2, 512], BF16); bCb = Buf()
        nn = sb("nn", [128, 2], F32); bnn = Buf()
        nb = sb("nb", [128, 2], BF16); bnb = Buf()
        qkT = sb("qkT", [128, 4, 64], BF16); bqkT = Buf()
        lfT = sb("lfT", [64, 64], F32); blfT = Buf()
        bias_s = sb("bias_s", [64, 1], F32); bbias = Buf()
        DT = sb("DT", [64, 64], F32); bDT = Buf()
        dq = sb("dq", [64, 1], F32); bdq = Buf()
        ws = sb("ws", [64, 1], F32); bws = Buf()
        dcs = sb("dcs", [128, 1], F32); bdcs = Buf()
        PT = sb("PT", [64, 64], BF16); bPT = Buf()
        a_sb = sb("a_sb", [64, 512], F32); ba = Buf()
        num = sb("num", [64, 512], F32); bnum = Buf()
        dd = sb("dd", [64, 2], F32); bdd = Buf()
        den = sb("den", [64, 1], F32); bden = Buf()
        kw = sb("kw", [64, 256], BF16); bkw = Buf()
        pTq = ps("pTq", [128, 8, 128], BF16); bpTq = Buf(True)
        pG = ps("pG", [128, 512], F32); bpG = Buf(True)
        pS = ps("pS", [128, 512], F32); bpS = Buf(True)
        pA = ps("pA", [128, 512], F32); bpA = Buf(True)
        pB = ps("pB", [128, 512], F32); bpB = Buf(True)
        pC = [ps("pC%d" % i, [128, 512], F32) for i in range(2)]; bpC = [Buf(True), Buf(True)]
        pm = ps("pm", [128, 512], F32); bpm = Buf(True)
        bh_s = Buf()

        for di in range(2):
            S.emit("vector", lambda e: e.memset(C[:], 0.0), writes=[bC])
            S.emit("vector", lambda e: e.memset(Cb[:], 0.0), writes=[bCb])
            S.emit("vector", lambda e: e.memset(nn[:], 0.0), writes=[bnn])
            S.emit("vector", lambda e: e.memset(nb[:], 0.0), writes=[bnb])
            nsc = nchunks // SC
            order = list(range(nsc)) if di == 0 else list(range(nsc - 1, -1, -1))

            def load(k):
                sc = order[k]; b = k % 2
                r0 = sc * SC * 64; r1 = r0 + SC * 64
                for src, dst, bd in ((mq_s, qs[b], bqs[b]), (mk_s, ks[b], bks[b]), (mv_s, vs[b], bvs[b]), (mg_s, gs[b], bgs[b])):
                    S.emit("sync", lambda e, src=src, dst=dst, r0=r0, r1=r1: e.dma_start(out=dst[:], in_=src[r0:r1, :].rearrange("(a p) c -> p a c", p=64)),
                           writes=[bd], dma=True)

            load(0)
            for k in range(nsc):
                sc = order[k]; b = k % 2
                if k + 1 < nsc:
                    load(k + 1)
                cl = list(range(SC)) if di == 0 else list(range(SC - 1, -1, -1))
                for a in cl:
                    q_c = qs[b][:, a, :]; k_c = ks[b][:, a, :]; v_c = vs[b][:, a, :]
                    ig = gs[b][:, a, 2 * di:2 * di + 1]; lf = gs[b][:, a, 2 * di + 1:2 * di + 2]
                    for j, src in enumerate((q_c[:, 0:128], q_c[:, 128:256], k_c[:, 0:128], k_c[:, 128:256])):
                        S.emit("tensor", lambda e, j=j, src=src: e.transpose(out=pTq[:, j, 0:64], in_=src, identity=ident[0:64, 0:64]),
                               reads=[bqs[b], bks[b], bid], writes=[bpTq])
                    S.emit("scalar", lambda e: e.activation(out=qkT[:], in_=pTq[:, 0:4, 0:64], func=AF.Copy), reads=[bpTq], writes=[bqkT])
                    S.emit("gpsimd", lambda e, lf=lf, di=di: e.tensor_scalar(out=lfT[:], in0=tri[:, di, 0, :], scalar1=lf, scalar2=None, op0=ALU.mult),
                           reads=[bgs[b], btri], writes=[blfT])
                    S.emit("tensor", lambda e: e.matmul(pG[0:64, 0:64], lhsT=ones_f[:, 0:64], rhs=lfT[:], start=True, stop=False),
                           reads=[bof, blfT], writes=[bpG])
                    S.emit("tensor", lambda e, di=di: e.matmul(pG[0:64, 0:64], lhsT=identf[0:64, 0:64], rhs=tri[:, di, 1, :], start=False, stop=True),
                           reads=[bid, btri], writes=[bpG])
                    S.emit("tensor", lambda e, lf=lf, di=di: e.matmul(pG[0:64, 64:65], lhsT=tri[:, di, 0, :], rhs=lf, start=True, stop=True),
                           reads=[btri, bgs[b]], writes=[bpG])
                    S.emit("tensor", lambda e, lf=lf: e.matmul(pG[:, 65:66], lhsT=ones_f[:], rhs=lf, start=True, stop=True),
                           reads=[bof, bgs[b]], writes=[bpG])
                    S.emit("vector", lambda e, ig=ig: e.tensor_tensor(out=bias_s[:], in0=ig, in1=pG[0:64, 64:65], op=ALU.subtract),
                           reads=[bgs[b], bpG], writes=[bbias])
                    S.emit("scalar", lambda e: e.activation(out=DT[:], in_=pG[0:64, 0:64], func=AF.Exp, bias=bias_s[:], scale=1.0), reads=[bpG, bbias], writes=[bDT])
                    S.emit("scalar", lambda e: e.activation(out=dq[:], in_=pG[0:64, 64:65], func=AF.Exp), reads=[bpG], writes=[bdq])
                    S.emit("scalar", lambda e: e.activation(out=ws[:], in_=pG[0:64, 65:66], func=AF.Exp, bias=bias_s[:], scale=1.0), reads=[bpG, bbias], writes=[bws])
                    S.emit("scalar", lambda e: e.activation(out=dcs[:], in_=pG[:, 65:66], func=AF.Exp), reads=[bpG], writes=[bdcs])
                    for dc in range(2):
                        S.emit("tensor", lambda e, dc=dc: e.matmul(pS[0:64, 0:64], lhsT=qkT[:, 2 + dc, :], rhs=qkT[:, dc, :], start=(dc == 0), stop=(dc == 1)),
                               reads=[bqkT], writes=[bpS])
                    S.emit("vector", lambda e: e.tensor_tensor(out=PT[:], in0=pS[0:64, 0:64], in1=DT[:], op=ALU.mult), reads=[bpS, bDT], writes=[bPT])
                    S.emit("tensor", lambda e, v_c=v_c: e.matmul(pA[0:64, :], lhsT=PT[:], rhs=v_c, start=True, stop=True), reads=[bPT, bvs[b]], writes=[bpA])
                    S.emit("tensor", lambda e: e.matmul(pm[0:64, 0:1], lhsT=PT[:], rhs=ones_b[:], start=True, stop=True), reads=[bPT, bob], writes=[bpm])
                    for dc in range(2):
                        S.emit("tensor", lambda e, dc=dc: e.matmul(pB[0:64, :], lhsT=qkT[:, dc, :], rhs=Cb[:, dc, :], start=(dc == 0), stop=(dc == 1)),
                               reads=[bqkT, bCb], writes=[bpB])
                    for dc in range(2):
                        S.emit("tensor", lambda e, dc=dc: e.matmul(pm[0:64, 1:2], lhsT=qkT[:, dc, :], rhs=nb[:, dc:dc + 1], start=(dc == 0), stop=(dc == 1)),
                               reads=[bqkT, bnb], writes=[bpm])
                    S.emit("scalar", lambda e: e.activation(out=a_sb[:], in_=pA[0:64, :], func=AF.Copy), reads=[bpA], writes=[ba])
                    S.emit("vector", lambda e: e.scalar_tensor_tensor(out=num[:], in0=pB[0:64, :], scalar=dq[:, 0:1], in1=a_sb[:], op0=ALU.mult, op1=ALU.add),
                           reads=[bpB, bdq, ba], writes=[bnum])
                    S.emit("vector", lambda e: e.tensor_copy(out=dd[:], in_=pm[0:64, 0:2]), reads=[bpm], writes=[bdd])
                    S.emit("vector", lambda e: e.scalar_tensor_tensor(out=den[:], in0=dd[:, 1:2], scalar=dq[:, 0:1], in1=dd[:, 0:1], op0=ALU.mult, op1=ALU.add),
                           reads=[bdd, bdq], writes=[bden])
                    S.emit("vector", lambda e: e.tensor_scalar(out=dd[:, 0:1], in0=den[:], scalar1=-1.0, scalar2=None, op0=ALU.mult), reads=[bden], writes=[bdd])
                    S.emit("vector", lambda e: e.tensor_tensor(out=den[:], in0=den[:], in1=dd[:, 0:1], op=ALU.max), reads=[bdd], writes=[bden])
                    S.emit("vector", lambda e: e.tensor_scalar(out=den[:], in0=den[:], scalar1=1.0, scalar2=None, op0=ALU.max), reads=[bden], writes=[bden])
                    S.emit("vector", lambda e: e.reciprocal(out=den[:], in_=den[:]), reads=[bden], writes=[bden])
                    S.emit("vector", lambda e, a=a, b=b: e.tensor_scalar(out=hs[b][:, a, :], in0=num[:], scalar1=den[:, 0:1], scalar2=None, op0=ALU.mult),
                           reads=[bnum, bden], writes=[bhs[b]])
                    S.emit("gpsimd", lambda e, k_c=k_c: e.tensor_scalar(out=kw[:], in0=k_c, scalar1=ws[:, 0:1], scalar2=None, op0=ALU.mult),
                           reads=[bks[b], bws], writes=[bkw])
                    for dc in range(2):
                        S.emit("tensor", lambda e, dc=dc, v_c=v_c: e.matmul(pC[dc][:], lhsT=kw[:, dc * 128:(dc + 1) * 128], rhs=v_c, start=True, stop=True),
                               reads=[bkw, bvs[b]], writes=[bpC[dc]])
                    for dc in range(2):
                        S.emit("tensor", lambda e, dc=dc: e.matmul(pm[:, 2 + dc:3 + dc], lhsT=kw[:, dc * 128:(dc + 1) * 128], rhs=ones_b[:], start=True, stop=True),
                               reads=[bkw, bob], writes=[bpm])
                    for dc in range(2):
                        S.emit("vector", lambda e, dc=dc: e.scalar_tensor_tensor(out=C[:, dc, :], in0=C[:, dc, :], scalar=dcs[:, 0:1], in1=pC[dc][:], op0=ALU.mult, op1=ALU.add),
                               reads=[bdcs, bpC[dc]], writes=[bC])
                    S.emit("scalar", lambda e: e.activation(out=Cb[:], in_=C[:], func=AF.Copy), reads=[bC], writes=[bCb])
                    S.emit("vector", lambda e: e.scalar_tensor_tensor(out=nn[:], in0=nn[:], scalar=dcs[:, 0:1], in1=pm[:, 2:4], op0=ALU.mult, op1=ALU.add),
                           reads=[bdcs, bpm], writes=[bnn])
                    S.emit("vector", lambda e: e.tensor_copy(out=nb[:], in_=nn[:]), reads=[bnn], writes=[bnb])
                r0 = sc * SC * 64; r1 = r0 + SC * 64
                S.emit("scalar", lambda e, b=b, r0=r0, r1=r1, di=di: e.dma_start(out=h_s[di, r0:r1, :].rearrange("(a p) c -> p a c", p=64), in_=hs[b][:]),
                       reads=[bhs[b]], writes=[bh_s], dma=True)
        S.run_phase()


def phase_ml_out(nc, S, h_s, mo_s, hgain, mem_out):
    with contextlib.ExitStack() as st:
        sb, ps = mkalloc(nc, st)
        hg = sb("hg", [128, 512], F32); bhg = Buf()
        S.emit("sync", lambda e: e.dma_start(out=hg[:], in_=hgain), writes=[bhg], dma=True)
        epsb = sb("epsb", [128, 1], F32); beps = Buf()
        S.emit("vector", lambda e: e.memset(epsb[:], EPS), writes=[beps])
        A = [[sb("hA%d_%d" % (i, j), [128, 512], F32) for j in range(3)] for i in range(2)]
        bA = [[Buf() for j in range(3)] for i in range(2)]
        sq = sb("sq", [128, 512], F32); bsq = Buf()
        ss = sb("ss", [128, 1], F32); bss = Buf()
        for t in range(NT):
            b = t % 2
            r0, r1 = t * 128, (t + 1) * 128
            for j, src in enumerate((h_s[0], h_s[1], mo_s)):
                S.emit("sync", lambda e, j=j, src=src, b=b, r0=r0, r1=r1: e.dma_start(out=A[b][j][:], in_=src[r0:r1, :]), writes=[bA[b][j]], dma=True)
            S.emit("vector", lambda e, b=b: e.tensor_tensor(out=A[b][0][:], in0=A[b][0][:], in1=A[b][1][:], op=ALU.add), reads=[bA[b][1]], writes=[bA[b][0]])
            S.emit("scalar", lambda e, b=b: e.activation(out=sq[:], in_=A[b][0][:], func=AF.Square), reads=[bA[b][0]], writes=[bsq])
            S.emit("vector", lambda e: e.reduce_sum(out=ss[:], in_=sq[:], axis=AX.X), reads=[bsq], writes=[bss])
            S.emit("scalar", lambda e: e.activation(out=ss[:], in_=ss[:], func=AF.Sqrt, scale=1.0 / 512, bias=epsb[:]), reads=[bss, beps], writes=[bss])
            S.emit("vector", lambda e: e.reciprocal(out=ss[:], in_=ss[:]), reads=[bss], writes=[bss])
            S.emit("vector", lambda e, b=b: e.scalar_tensor_tensor(out=A[b][0][:], in0=A[b][0][:], scalar=ss[:, 0:1], in1=hg[:], op0=ALU.mult, op1=ALU.mult),
                   reads=[bss, bhg], writes=[bA[b][0]])
            S.emit("gpsimd", lambda e, b=b: e.tensor_tensor(out=A[b][1][:], in0=A[b][0][:], in1=A[b][2][:], op=ALU.mult), reads=[bA[b][0], bA[b][2]], writes=[bA[b][1]])
            S.emit("sync", lambda e, b=b, r0=r0, r1=r1: e.dma_start(out=mem_out[r0:r1, :], in_=A[b][1][:]), reads=[bA[b][1]], dma=True)
        S.run_phase()


def ml_consts():
    r = np.arange(64)[:, None]; t = np.arange(64)[None, :]
    tri = np.zeros((64, 2, 2, 64), np.float32)
    tri[:, 0, 0, :] = (r <= t)
    tri[:, 1, 0, :] = (r >= t)
    tri[:, 0, 1, :] = np.where(r <= t, 0.0, -30000.0)
    tri[:, 1, 1, :] = np.where(r >= t, 0.0, -30000.0)
    return tri


TPC = 1024
HALF = 256


def norm_transpose(S, sb_objs, src_dram, r0, gain, bgain, dstT, bdstT, col0, nchunks, normalize):
    (xb, bxb, sq, bsq, ss, bss, epsb, beps, xs, bxs, pT, bpT, ident, bid, cnt) = sb_objs
    F = nchunks * 128
    i = cnt[0] % 2
    cnt[0] += 1
    S.emit("sync", lambda e: e.dma_start(out=xb[i][:, 0:F], in_=src_dram[r0:r0 + 128, :]), writes=[bxb[i]], dma=True)
    if normalize:
        S.emit("scalar", lambda e: e.activation(out=sq[:, 0:F], in_=xb[i][:, 0:F], func=AF.Square), reads=[bxb[i]], writes=[bsq])
        S.emit("vector", lambda e: e.reduce_sum(out=ss[:], in_=sq[:, 0:F], axis=AX.X), reads=[bsq], writes=[bss])
        S.emit("scalar", lambda e: e.activation(out=ss[:], in_=ss[:], func=AF.Sqrt, scale=1.0 / F, bias=epsb[:]), reads=[bss, beps], writes=[bss])
        S.emit("vector", lambda e: e.reciprocal(out=ss[:], in_=ss[:]), reads=[bss], writes=[bss])
        S.emit("vector", lambda e: e.tensor_scalar(out=xs[:, 0:F], in0=xb[i][:, 0:F], scalar1=ss[:, 0:1], scalar2=None, op0=ALU.mult),
               reads=[bxb[i], bss], writes=[bxs])
    else:
        S.emit("vector", lambda e: e.tensor_copy(out=xs[:, 0:F], in_=xb[i][:, 0:F]), reads=[bxb[i]], writes=[bxs])
    for c8 in range(nchunks // 8):
        p = pT[c8 % 2]; bp = bpT[c8 % 2]
        for cc in range(8):
            c = c8 * 8 + cc
            S.emit("tensor", lambda e, p=p, cc=cc, c=c: e.transpose(out=p[:, cc, :], in_=xs[:, c * 128:(c + 1) * 128], identity=ident[:]),
                   reads=[bxs, bid], writes=[bp])
        dv = dstT[:, c8 * 8:(c8 + 1) * 8, col0:col0 + 128]
        if gain is not None:
            S.emit("vector", lambda e, p=p, dv=dv, c8=c8: e.tensor_tensor(out=dv, in0=p[:], in1=gain[:, c8 * 8:(c8 + 1) * 8].unsqueeze(2).to_broadcast([128, 8, 128]),
                                                                         op=ALU.mult), reads=[bp, bgain], writes=[bdstT])
        elif c8 % 2 == 0:
            S.emit("scalar", lambda e, p=p, dv=dv: e.activation(out=dv, in_=p[:], func=AF.Copy), reads=[bp], writes=[bdstT])
        else:
            S.emit("vector", lambda e, p=p, dv=dv: e.tensor_copy(out=dv, in_=p[:]), reads=[bp], writes=[bdstT])


def nt_objs(S, sb, ps):
    xb = [sb("xb%d" % i, [128, DM], F32) for i in range(2)]
    bxb = [Buf(), Buf()]
    sq = sb("sq", [128, DM], BF16); bsq = Buf()
    ss = sb("ss", [128, 1], F32); bss = Buf()
    epsb = sb("epsb", [128, 1], F32); beps = Buf()
    S.emit("vector", lambda e: e.memset(epsb[:], EPS), writes=[beps])
    xs = sb("xs", [128, DM], BF16); bxs = Buf()
    pT = [ps("pT%d" % i, [128, 8, 128], BF16) for i in range(2)]
    bpT = [Buf(True), Buf(True)]
    ident, identf, bid = make_ident(S, sb)
    return (xb, bxb, sq, bsq, ss, bss, epsb, beps, xs, bxs, pT, bpT, ident, bid, [0]), identf


def phase_l2(nc, S, x, att, mem, g1t, bab, wgt, wat, wmt, wot, x1_out):
    with contextlib.ExitStack() as st:
        sb, ps = mkalloc(nc, st)
        objs, identf = nt_objs(S, sb, ps)
        g1 = sb("g1", [128, 32], F32); bg1 = Buf()
        S.emit("sync", lambda e: e.dma_start(out=g1[:], in_=g1t), writes=[bg1], dma=True)
        bb = sb("bb", [128, 2, 32], F32); bbb = Buf()
        S.emit("sync", lambda e: e.dma_start(out=bb[:], in_=bab), writes=[bbb], dma=True)
        hT = sb("hT", [128, 32, HALF], BF16); bhT = Buf()
        aT = sb("aT", [128, 8, HALF], BF16); baT = Buf()
        mT = sb("mT", [128, 32, HALF], BF16); bmT = Buf()
        mg = sb("mg", [128, 32, HALF], BF16); bmg = Buf()
        wga = [sb("wga%d" % i, [128, 32, 128], BF16) for i in range(2)]; bwga = [Buf(), Buf()]
        wgm = [sb("wgm%d" % i, [128, 32, 128], BF16) for i in range(2)]; bwgm = [Buf(), Buf()]
        wa = [sb("wa%d" % i, [128, 8, 128], BF16) for i in range(2)]; bwa = [Buf(), Buf()]
        wm = [sb("wm%d" % i, [128, 32, 128], BF16) for i in range(2)]; bwm = [Buf(), Buf()]
        pg = [ps("pg%d" % i, [128, 512], F32) for i in range(4)]; bpg = [Buf(True) for _ in range(4)]
        sa = sb("sa", [128, HALF], F32); bsa = Buf()
        sm = sb("sm", [128, HALF], F32); bsm = Buf()
        t1 = sb("t1", [128, HALF], F32); bt1 = Buf()
        t2 = sb("t2", [128, HALF], F32); bt2 = Buf()
        xo = [sb("xo%d" % i, [128, HALF], F32) for i in range(2)]; bxo = [Buf(), Buf()]
        for hf in range(TPC // HALF):
            t0 = hf * HALF
            for tt in range(HALF // 128):
                norm_transpose(S, objs, x, t0 + tt * 128, g1, bg1, hT, bhT, tt * 128, 32, True)
                norm_transpose(S, objs, att, t0 + tt * 128, None, None, aT, baT, tt * 128, 8, False)
                norm_transpose(S, objs, mem, t0 + tt * 128, None, None, mT, bmT, tt * 128, 32, False)

            def loadw(n):
                i = n % 2
                S.emit("gpsimd", lambda e: e.dma_start(out=wga[i][:], in_=wgt[0, n]), writes=[bwga[i]], dma=True)
                S.emit("gpsimd", lambda e: e.dma_start(out=wgm[i][:], in_=wgt[1, n]), writes=[bwgm[i]], dma=True)
                S.emit("gpsimd", lambda e: e.dma_start(out=wa[i][:], in_=wat[n]), writes=[bwa[i]], dma=True)
                S.emit("gpsimd", lambda e: e.dma_start(out=wm[i][:], in_=wmt[n]), writes=[bwm[i]], dma=True)

            loadw(0)
            for n in range(32):
                i = n % 2
                if n + 1 < 32:
                    loadw(n + 1)
                for c in range(32):
                    S.emit("tensor", lambda e, c=c, i=i: e.matmul(pg[0][:, 0:HALF], lhsT=wga[i][:, c, :], rhs=hT[:, c, :], start=(c == 0), stop=(c == 31)),
                           reads=[bwga[i], bhT], writes=[bpg[0]])
                for c in range(32):
                    S.emit("tensor", lambda e, c=c, i=i: e.matmul(pg[1][:, 0:HALF], lhsT=wgm[i][:, c, :], rhs=hT[:, c, :], start=(c == 0), stop=(c == 31)),
                           reads=[bwgm[i], bhT], writes=[bpg[1]])
                for c in range(8):
                    S.emit("tensor", lambda e, c=c, i=i: e.matmul(pg[2][:, 0:HALF], lhsT=wa[i][:, c, :], rhs=aT[:, c, :], start=(c == 0), stop=(c == 7)),
                           reads=[bwa[i], baT], writes=[bpg[2]])
                for c in range(32):
                    S.emit("tensor", lambda e, c=c, i=i: e.matmul(pg[3][:, 0:HALF], lhsT=wm[i][:, c, :], rhs=mT[:, c, :], start=(c == 0), stop=(c == 31)),
                           reads=[bwm[i], bmT], writes=[bpg[3]])
                S.emit("scalar", lambda e, n=n: e.activation(out=sa[:], in_=pg[0][:, 0:HALF], func=AF.Sigmoid, bias=bb[:, 0, n:n + 1], scale=1.0), reads=[bpg[0], bbb], writes=[bsa])
                S.emit("scalar", lambda e, n=n: e.activation(out=sm[:], in_=pg[1][:, 0:HALF], func=AF.Sigmoid, bias=bb[:, 1, n:n + 1], scale=1.0), reads=[bpg[1], bbb], writes=[bsm])
                S.emit("vector", lambda e: e.tensor_tensor(out=t1[:], in0=pg[2][:, 0:HALF], in1=sa[:], op=ALU.mult), reads=[bpg[2], bsa], writes=[bt1])
                S.emit("vector", lambda e: e.tensor_tensor(out=t2[:], in0=pg[3][:, 0:HALF], in1=sm[:], op=ALU.mult), reads=[bpg[3], bsm], writes=[bt2])
                S.emit("gpsimd", lambda e, n=n: e.tensor_tensor(out=mg[:, n, :], in0=t1[:], in1=t2[:], op=ALU.add), reads=[bt1, bt2], writes=[bmg])
            for cb in range(DM // HALF):
                S.emit("gpsimd", lambda e, cb=cb: e.dma_start(out=mT[:], in_=wot[cb]), writes=[bmT], dma=True)
                for tt in range(HALF // 128):
                    k = (cb * (HALF // 128) + tt) % 2
                    po = pg[k]; bpo = bpg[k]
                    r0 = t0 + tt * 128
                    S.emit("sync", lambda e, k=k, r0=r0, cb=cb: e.dma_start(out=xo[k][:], in_=x[r0:r0 + 128, cb * HALF:(cb + 1) * HALF]), writes=[bxo[k]], dma=True)
                    for c in range(32):
                        S.emit("tensor", lambda e, c=c, po=po, tt=tt: e.matmul(po[:, 0:HALF], lhsT=mg[:, c, tt * 128:(tt + 1) * 128], rhs=mT[:, c, :], start=(c == 0), stop=(c == 31)),
                               reads=[bmg, bmT], writes=[bpo])
                    S.emit("vector", lambda e, k=k, po=po: e.tensor_tensor(out=xo[k][:], in0=po[:, 0:HALF], in1=xo[k][:], op=ALU.add), reads=[bpo], writes=[bxo[k]])
                    S.emit("sync", lambda e, k=k, r0=r0, cb=cb: e.dma_start(out=x1_out[r0:r0 + 128, cb * HALF:(cb + 1) * HALF], in_=xo[k][:]), reads=[bxo[k]], dma=True)
        S.run_phase()


def l2_weights(inp):
    f32 = np.float32
    w_in = inp["w_in"][0]
    wg = w_in[:, 29728 - 8192:]
    wgt = np.ascontiguousarray(wg.reshape(32, 128, 2, 32, 128).transpose(2, 3, 1, 0, 4), dtype=f32)
    wat = np.ascontiguousarray(inp["w_attn_branch"][0].reshape(8, 128, 32, 128).transpose(2, 1, 0, 3), dtype=f32)
    wmt = np.ascontiguousarray(inp["w_mlstm_branch"][0].reshape(32, 128, 32, 128).transpose(2, 1, 0, 3), dtype=f32)
    wot = np.ascontiguousarray(inp["w_out"][0].reshape(32, 128, DM // HALF, HALF).transpose(2, 1, 0, 3), dtype=f32)
    bab = np.ascontiguousarray(inp["branch_gate_bias"][0].reshape(2, 32, 128).transpose(2, 0, 1), dtype=f32)
    g1t = np.ascontiguousarray(inp["norm1_gain"][0].reshape(32, 128).T, dtype=f32)
    return {"wgt": wgt, "wat": wat, "wmt": wmt, "wot": wot, "bab": bab, "g1t": g1t}


def build_l2():
    nc = bass.Bass("TRN2", target_bir_lowering=False)
    x = nc.dram_tensor("x", [TPC, DM], F32, kind="ExternalInput").ap()
    att = nc.dram_tensor("att", [TPC, 1024], F32, kind="ExternalInput").ap()
    mem = nc.dram_tensor("mem", [TPC, DM], F32, kind="ExternalInput").ap()
    g1t = nc.dram_tensor("g1t", [128, 32], F32, kind="ExternalInput").ap()
    bab = nc.dram_tensor("bab", [128, 2, 32], F32, kind="ExternalInput").ap()
    wgt = nc.dram_tensor("wgt", [2, 32, 128, 32, 128], F32, kind="ExternalInput").ap()
    wat = nc.dram_tensor("wat", [32, 128, 8, 128], F32, kind="ExternalInput").ap()
    wmt = nc.dram_tensor("wmt", [32, 128, 32, 128], F32, kind="ExternalInput").ap()
    wot = nc.dram_tensor("wot", [DM // HALF, 128, 32, HALF], F32, kind="ExternalInput").ap()
    x1 = nc.dram_tensor("x1", [TPC, DM], F32, kind="ExternalOutput").ap()
    with contextlib.ExitStack() as gst:
        S = Sched(nc, gst)
        phase_l2(nc, S, x, att, mem, g1t, bab, wgt, wat, wmt, wot, x1)
    return nc


TB = 512
DE = 768
NF = DE // 128
BIG = 1.0e9


def phase_moe(nc, S, x1, g2t, wrt, brr, wgt, wut, wd, y_out, nblocks=SEQ // TB, nexp=8):
    with contextlib.ExitStack() as st:
        sb, ps = mkalloc(nc, st)
        xb0 = sb("xb", [128, DM], F32); bxb0 = Buf()
        xs = sb("xs", [128, DM], BF16); bxs = Buf()
        ss = sb("ss", [128, 1], F32); bss = Buf()
        epsb = sb("epsb", [128, 1], F32); beps = Buf()
        S.emit("vector", lambda e: e.memset(epsb[:], EPS), writes=[beps])
        pT = [ps("pT%d" % i, [128, 8, 128], BF16) for i in range(2)]
        bpT = [Buf(True), Buf(True)]
        ident, identf, bid = make_ident(S, sb)
        objs = ([xb0, xb0], [bxb0, bxb0], xs, bxs, ss, bss, epsb, beps, xs, bxs, pT, bpT, ident, bid, [0])
        g2 = sb("g2", [128, 32], F32); bg2 = Buf()
        S.emit("sync", lambda e: e.dma_start(out=g2[:], in_=g2t), writes=[bg2], dma=True)
        wr = sb("wr", [128, 32, 72], BF16); bwr = Buf()
        S.emit("gpsimd", lambda e: e.dma_start(out=wr[:], in_=wrt), writes=[bwr], dma=True)
        br = sb("br", [128, 72], F32); bbr = Buf()
        S.emit("sync", lambda e: e.dma_start(out=br[:], in_=brr), writes=[bbr], dma=True)
        ones8 = sb("ones8", [8, 128], F32); bo8 = Buf()
        S.emit("gpsimd", lambda e: e.memset(ones8[:], 1.0), writes=[bo8])
        sel = [sb("sel%d" % i, [8, 128], F32) for i in range(2)]; bsel = [Buf(), Buf()]
        h2T = sb("h2T", [128, 32, TB], BF16); bh2T = Buf()
        acc = sb("acc", [128, TB // 128, DM], F32); bacc = [Buf() for _ in range(TB // 128)]
        wgb = [sb("wgb%d" % i, [128, 32, 128], BF16) for i in range(2)]; bwgb = [Buf(), Buf()]
        wub = [sb("wub%d" % i, [128, 32, 128], BF16) for i in range(2)]; bwub = [Buf(), Buf()]
        wdb = [sb("wdb%d" % i, [128, NF, 512], BF16) for i in range(2)]; bwdb = [Buf(), Buf()]
        aT = sb("aT", [128, NF, TB], BF16); baT = Buf()
        sg = sb("sg", [128, TB], F32); bsg = Buf()
        tg = sb("tg", [128, TB], F32); btg = Buf()
        wbc = sb("wbc", [128, TB], F32); bwbc = Buf()
        wT = sb("wT", [8, TB], F32); bwT = Buf()
        lg = sb("lg", [128, 72], F32); blg = Buf()
        m = sb("m", [128, 8], F32); bm = Buf()
        ohg = sb("ohg", [128, 8], F32); bohg = Buf()
        eg = sb("eg", [128, 8], F32); beg = Buf()
        msk = sb("msk", [128, 64], F32); bmsk = Buf()
        oh1 = sb("oh1", [128, 64], F32); boh1 = Buf()
        oh2 = sb("oh2", [128, 64], F32); boh2 = Buf()
        wts = sb("wts", [128, 64], F32); bwts = Buf()
        pr = ps("pr", [128, 512], F32); bpr = Buf(True)
        pwb = ps("pwb", [128, 512], F32); bpwb = Buf(True)
        pgt = ps("pgt", [128, 512], F32); bpgt = Buf(True)
        put = ps("put", [128, 512], F32); bput = Buf(True)
        py = [ps("py%d" % i, [128, 512], F32) for i in range(2)]; bpy = [Buf(True), Buf(True)]
        wcnt = [0]
        dcnt = [0]
        ycnt = [0]

        for tb in range(nblocks):
            t0 = tb * TB
            for tt in range(TB // 128):
                norm_transpose(S, objs, x1, t0 + tt * 128, g2, bg2, h2T, bh2T, tt * 128, 32, True)
            for tt in range(TB // 128):
                S.emit("vector", lambda e, tt=tt: e.memset(acc[:, tt, :], 0.0), writes=[bacc[tt]])
            for tt in range(TB // 128):
                for c in range(32):
                    S.emit("tensor", lambda e, c=c, tt=tt: e.matmul(pr[:, 0:72], lhsT=h2T[:, c, tt * 128:(tt + 1) * 128], rhs=wr[:, c, :], start=(c == 0), stop=(c == 31)),
                           reads=[bh2T, bwr], writes=[bpr])
                V = "vector"
                S.emit(V, lambda e: e.tensor_tensor(out=lg[:], in0=pr[:, 0:72], in1=br[:], op=ALU.add), reads=[bpr, bbr], writes=[blg])
                S.emit(V, lambda e: e.reduce_max(out=m[:, 0:1], in_=lg[:, 0:8], axis=AX.X), reads=[blg], writes=[bm])
                S.emit(V, lambda e: e.tensor_scalar(out=ohg[:], in0=lg[:, 0:8], scalar1=m[:, 0:1], scalar2=None, op0=ALU.is_equal), reads=[blg, bm], writes=[bohg])
                S.emit(V, lambda e: e.tensor_scalar(out=m[:, 1:2], in0=m[:, 0:1], scalar1=-1.0, scalar2=None, op0=ALU.mult), reads=[bm], writes=[bm])
                S.emit("scalar", lambda e: e.activation(out=eg[:], in_=lg[:, 0:8], func=AF.Exp, bias=m[:, 1:2], scale=1.0), reads=[blg, bm], writes=[beg])
                S.emit(V, lambda e: e.reduce_sum(out=m[:, 2:3], in_=eg[:], axis=AX.X), reads=[beg], writes=[bm])
                S.emit(V, lambda e: e.reciprocal(out=m[:, 2:3], in_=m[:, 2:3]), reads=[bm], writes=[bm])
                S.emit(V, lambda e: e.tensor_scalar(out=ohg[:], in0=ohg[:], scalar1=-1.0, scalar2=BIG, op0=ALU.add, op1=ALU.mult), reads=[bohg], writes=[bohg])
                S.emit(V, lambda e: e.tensor_tensor(out=msk[:].rearrange("p (g k) -> p g k", g=8), in0=lg[:, 8:72].rearrange("p (g k) -> p g k", g=8),
                                                    in1=ohg[:].unsqueeze(2).to_broadcast([128, 8, 8]), op=ALU.add), reads=[blg, bohg], writes=[bmsk])
                S.emit(V, lambda e: e.reduce_max(out=m[:, 3:4], in_=msk[:], axis=AX.X), reads=[bmsk], writes=[bm])
                S.emit(V, lambda e: e.tensor_scalar(out=oh1[:], in0=msk[:], scalar1=m[:, 3:4], scalar2=None, op0=ALU.is_equal), reads=[bmsk, bm], writes=[boh1])
                S.emit(V, lambda e: e.scalar_tensor_tensor(out=msk[:], in0=oh1[:], scalar=-BIG, in1=msk[:], op0=ALU.mult, op1=ALU.add), reads=[boh1], writes=[bmsk])
                S.emit(V, lambda e: e.reduce_max(out=m[:, 4:5], in_=msk[:], axis=AX.X), reads=[bmsk], writes=[bm])
                S.emit(V, lambda e: e.tensor_scalar(out=oh2[:], in0=msk[:], scalar1=m[:, 4:5], scalar2=None, op0=ALU.is_equal), reads=[bmsk, bm], writes=[boh2])
                S.emit(V, lambda e: e.tensor_tensor(out=m[:, 5:6], in0=m[:, 4:5], in1=m[:, 3:4], op=ALU.subtract), reads=[bm], writes=[bm])
                S.emit("scalar", lambda e: e.activation(out=m[:, 5:6], in_=m[:, 5:6], func=AF.Exp), reads=[bm], writes=[bm])
                S.emit(V, lambda e: e.tensor_scalar(out=m[:, 5:6], in0=m[:, 5:6], scalar1=1.0, scalar2=None, op0=ALU.add), reads=[bm], writes=[bm])
                S.emit(V, lambda e: e.reciprocal(out=m[:, 5:6], in_=m[:, 5:6]), reads=[bm], writes=[bm])
                S.emit(V, lambda e: e.tensor_tensor(out=m[:, 6:7], in0=m[:, 5:6], in1=m[:, 2:3], op=ALU.mult), reads=[bm], writes=[bm])
                S.emit(V, lambda e: e.tensor_tensor(out=m[:, 7:8], in0=m[:, 2:3], in1=m[:, 6:7], op=ALU.subtract), reads=[bm], writes=[bm])
                S.emit(V, lambda e: e.tensor_scalar(out=wts[:], in0=oh1[:], scalar1=m[:, 6:7], scalar2=None, op0=ALU.mult), reads=[boh1, bm], writes=[bwts])
                S.emit(V, lambda e: e.scalar_tensor_tensor(out=wts[:], in0=oh2[:], scalar=m[:, 7:8], in1=wts[:], op0=ALU.mult, op1=ALU.add), reads=[boh2, bm], writes=[bwts])
                S.emit("tensor", lambda e: e.transpose(out=pr[0:8, 128:256], in_=wts[:, 0:8], identity=identf[:]), reads=[bwts, bid], writes=[bpr])
                S.emit(V, lambda e, tt=tt: e.tensor_copy(out=wT[:, tt * 128:(tt + 1) * 128], in_=pr[0:8, 128:256]), reads=[bpr], writes=[bwT])
            steps = []
            for ex in range(nexp):
                for f in range(NF):
                    steps.append(("gu", ex, f))
                for cb in range(DM // 512):
                    steps.append(("dn", ex, cb))

            def prefetch(si_):
                kind, ex, k = steps[si_]
                if kind == "gu":
                    wi = wcnt[0] % 2
                    wcnt[0] += 1
                    S.emit("gpsimd", lambda e: e.dma_start(out=wgb[wi][:], in_=wgt[ex, k]), writes=[bwgb[wi]], dma=True)
                    S.emit("gpsimd", lambda e: e.dma_start(out=wub[wi][:], in_=wut[ex, k]), writes=[bwub[wi]], dma=True)
                    return wi
                di = dcnt[0] % 2
                dcnt[0] += 1
                S.emit("gpsimd", lambda e: e.dma_start(out=wdb[di][:], in_=wd[ex].rearrange("(f p) n -> p f n", p=128)[:, :, k * 512:(k + 1) * 512]),
                       writes=[bwdb[di]], dma=True)
                return di

            bufidx = {0: prefetch(0)}
            for si_, (kind, ex, k) in enumerate(steps):
                if si_ + 1 < len(steps):
                    bufidx[si_ + 1] = prefetch(si_ + 1)
                bi = bufidx[si_]
                if kind == "gu":
                    f = k
                    if f == 0:
                        s2 = ex % 2
                        S.emit("gpsimd", lambda e, s2=s2, ex=ex: e.affine_select(out=sel[s2][:], in_=ones8[:], pattern=[[0, 128]], compare_op=ALU.is_equal,
                                                                                 fill=0.0, base=-ex, channel_multiplier=1), reads=[bo8], writes=[bsel[s2]])
                        S.emit("tensor", lambda e, s2=s2: e.matmul(pwb[:, 0:TB], lhsT=sel[s2][:], rhs=wT[:], start=True, stop=True), reads=[bsel[s2], bwT], writes=[bpwb])
                        S.emit("scalar", lambda e: e.activation(out=wbc[:], in_=pwb[:, 0:TB], func=AF.Copy), reads=[bpwb], writes=[bwbc])
                    for c in range(32):
                        S.emit("tensor", lambda e, c=c, bi=bi: e.matmul(pgt[:, 0:TB], lhsT=wgb[bi][:, c, :], rhs=h2T[:, c, :], start=(c == 0), stop=(c == 31)),
                               reads=[bwgb[bi], bh2T], writes=[bpgt])
                    for c in range(32):
                        S.emit("tensor", lambda e, c=c, bi=bi: e.matmul(put[:, 0:TB], lhsT=wub[bi][:, c, :], rhs=h2T[:, c, :], start=(c == 0), stop=(c == 31)),
                               reads=[bwub[bi], bh2T], writes=[bput])
                    S.emit("scalar", lambda e: e.activation(out=sg[:], in_=pgt[:, 0:TB], func=AF.Silu), reads=[bpgt], writes=[bsg])
                    S.emit("vector", lambda e: e.tensor_tensor(out=tg[:], in0=put[:, 0:TB], in1=sg[:], op=ALU.mult), reads=[bput, bsg], writes=[btg])
                    S.emit("vector", lambda e, f=f: e.tensor_tensor(out=aT[:, f, :], in0=tg[:], in1=wbc[:], op=ALU.mult), reads=[btg, bwbc], writes=[baT])
                else:
                    cb = k
                    for tt in range(TB // 128):
                        yi = ycnt[0] % 2
                        ycnt[0] += 1
                        for f in range(NF):
                            S.emit("tensor", lambda e, f=f, tt=tt, yi=yi, bi=bi: e.matmul(py[yi][:], lhsT=aT[:, f, tt * 128:(tt + 1) * 128], rhs=wdb[bi][:, f, :],
                                                                                        start=(f == 0), stop=(f == NF - 1)),
                                   reads=[baT, bwdb[bi]], writes=[bpy[yi]])
                        S.emit("vector", lambda e, tt=tt, cb=cb, yi=yi: e.tensor_tensor(out=acc[:, tt, cb * 512:(cb + 1) * 512], in0=py[yi][:],
                                                                                      in1=acc[:, tt, cb * 512:(cb + 1) * 512], op=ALU.add),
                               reads=[bpy[yi]], writes=[bacc[tt]])
            for tt in range(TB // 128):
                r0 = t0 + tt * 128
                S.emit("sync", lambda e, tt=tt, r0=r0: e.dma_start(out=y_out[r0:r0 + 128, :], in_=acc[:, tt, :]), reads=[bacc[tt]], dma=True)
        S.run_phase()


def l3_inputs(inp, j, x1):
    f32 = np.float32
    perm = [(j + k) % 8 for k in range(8)]
    w_rg = inp["w_router_group"][0][:, perm]
    b_rg = inp["b_router_group"][0][perm]
    w_re = inp["w_router_expert"][0].reshape(DM, 8, 8)[:, perm, :].reshape(DM, 64)
    b_re = inp["b_router_expert"][0].reshape(8, 8)[perm].reshape(64)
    wrc = np.concatenate([w_rg, w_re], axis=1)
    wrt = np.ascontiguousarray(wrc.reshape(32, 128, 72).transpose(1, 0, 2), dtype=f32)
    brr = np.ascontiguousarray(np.broadcast_to(np.concatenate([b_rg, b_re])[None, :], (128, 72)), dtype=f32)
    wg = inp["w_expert_gate"][0][j * 8:(j + 1) * 8]
    wu = inp["w_expert_up"][0][j * 8:(j + 1) * 8]
    wgt = np.ascontiguousarray(wg.reshape(8, 32, 128, NF, 128).transpose(0, 3, 2, 1, 4), dtype=f32)
    wut = np.ascontiguousarray(wu.reshape(8, 32, 128, NF, 128).transpose(0, 3, 2, 1, 4), dtype=f32)
    wd = np.ascontiguousarray(inp["w_expert_down"][0][j * 8:(j + 1) * 8], dtype=f32)
    g2t = np.ascontiguousarray(inp["norm2_gain"][0].reshape(32, 128).T, dtype=f32)
    return {"x1": x1, "g2t": g2t, "wrt": wrt, "brr": brr, "wgt": wgt, "wut": wut, "wd": wd}


def build_l3(nblocks=SEQ // TB, nexp=8):
    nc = bass.Bass("TRN2", target_bir_lowering=False)
    ntok = nblocks * TB
    x1 = nc.dram_tensor("x1", [ntok, DM], F32, kind="ExternalInput").ap()
    g2t = nc.dram_tensor("g2t", [128, 32], F32, kind="ExternalInput").ap()
    wrt = nc.dram_tensor("wrt", [128, 32, 72], F32, kind="ExternalInput").ap()
    brr = nc.dram_tensor("brr", [128, 72], F32, kind="ExternalInput").ap()
    wgt = nc.dram_tensor("wgt", [8, NF, 128, 32, 128], F32, kind="ExternalInput").ap()
    wut = nc.dram_tensor("wut", [8, NF, 128, 32, 128], F32, kind="ExternalInput").ap()
    wd = nc.dram_tensor("wd", [8, DE, DM], F32, kind="ExternalInput").ap()
    y = nc.dram_tensor("y", [ntok, DM], F32, kind="ExternalOutput").ap()
    with contextlib.ExitStack() as gst:
        S = Sched(nc, gst)
        phase_moe(nc, S, x1, g2t, wrt, brr, wgt, wut, wd, y, nblocks, nexp)
    return nc


def phase_combine(nc, S, x1, ys, out, ntiles=8):
    with contextlib.ExitStack() as st:
        sb, ps = mkalloc(nc, st)
        CW = 1024
        A = [[sb("cA%d_%d" % (i, k), [128, CW], F32) for k in range(9)] for i in range(2)]
        bA = [[Buf() for k in range(9)] for i in range(2)]
        n = 0
        for tt in range(ntiles):
            for cc in range(DM // CW):
                i = n % 2
                n += 1
                r0 = tt * 128; c0 = cc * CW
                S.emit("sync", lambda e, i=i, r0=r0, c0=c0: e.dma_start(out=A[i][0][:], in_=x1[r0:r0 + 128, c0:c0 + CW]), writes=[bA[i][0]], dma=True)
                for k in range(8):
                    S.emit("sync", lambda e, i=i, k=k, r0=r0, c0=c0: e.dma_start(out=A[i][k + 1][:], in_=ys[k, r0:r0 + 128, c0:c0 + CW]), writes=[bA[i][k + 1]], dma=True)
                for k in range(8):
                    eng = "vector" if k % 2 == 0 else "gpsimd"
                    S.emit(eng, lambda e, i=i, k=k: e.tensor_tensor(out=A[i][0][:], in0=A[i][0][:], in1=A[i][k + 1][:], op=ALU.add), reads=[bA[i][k + 1]], writes=[bA[i][0]])
                S.emit("sync", lambda e, i=i, r0=r0, c0=c0: e.dma_start(out=out[r0:r0 + 128, c0:c0 + CW], in_=A[i][0][:]), reads=[bA[i][0]], dma=True)
        S.run_phase()


def build_l4():
    nc = bass.Bass("TRN2", target_bir_lowering=False)
    x1 = nc.dram_tensor("x1", [1024, DM], F32, kind="ExternalInput").ap()
    ys = nc.dram_tensor("ys", [8, 1024, DM], F32, kind="ExternalInput").ap()
    out = nc.dram_tensor("out", [1024, DM], F32, kind="ExternalOutput").ap()
    with contextlib.ExitStack() as gst:
        S = Sched(nc, gst)
        phase_combine(nc, S, x1, ys, out)
    return nc


def build_l1():
    nc = bass.Bass("TRN2", target_bir_lowering=False)
    x = nc.dram_tensor("x", [SEQ, DM], F32, kind="ExternalInput").ap()
    w_att = nc.dram_tensor("w_att", [DM, 1152], F32, kind="ExternalInput").ap()
    w_ml = nc.dram_tensor("w_ml", [DM, 1540], F32, kind="ExternalInput").ap()
    g1t = nc.dram_tensor("g1t", [128, 32], F32, kind="ExternalInput").ap()
    gqk = nc.dram_tensor("gqk", [128, 768], F32, kind="ExternalInput").ap()
    cs = nc.dram_tensor("cs", [SEQ, 128], F32, kind="ExternalInput").ap()
    masks = nc.dram_tensor("masks", [128, 3, 128], F32, kind="ExternalInput").ap()
    gbias = nc.dram_tensor("gbias", [128, 4], F32, kind="ExternalInput").ap()
    hgain = nc.dram_tensor("hgain", [128, 512], F32, kind="ExternalInput").ap()
    tri_d = nc.dram_tensor("tri_d", [64, 2, 2, 64], F32, kind="ExternalInput").ap()
    qkv_s = nc.dram_tensor("qkv_s", [3, 3, SEQ, 128], BF16, kind="Internal").ap()
    acc_s = nc.dram_tensor("acc_s", [3, SEQ, 132], F32, kind="Internal").ap()
    mq_s = nc.dram_tensor("mq_s", [SEQ, 256], BF16, kind="Internal").ap()
    mk_s = nc.dram_tensor("mk_s", [SEQ, 256], BF16, kind="Internal").ap()
    mv_s = nc.dram_tensor("mv_s", [SEQ, 512], BF16, kind="Internal").ap()
    mo_s = nc.dram_tensor("mo_s", [SEQ, 512], F32, kind="Internal").ap()
    mg_s = nc.dram_tensor("mg_s", [SEQ, 4], F32, kind="Internal").ap()
    h_s = nc.dram_tensor("h_s", [2, SEQ, 512], F32, kind="Internal").ap()
    att_out = nc.dram_tensor("att_out", [SEQ, 128], F32, kind="ExternalOutput").ap()
    mem_out = nc.dram_tensor("mem_out", [SEQ, 512], F32, kind="ExternalOutput").ap()
    with contextlib.ExitStack() as gst:
        S = Sched(nc, gst)
        phase_att_proj(nc, S, x, w_att, g1t, gqk, cs, qkv_s)
        phase_att_core(nc, S, qkv_s, masks, acc_s, att_out)
        phase_ml_proj(nc, S, x, w_ml, g1t, gbias, mq_s, mk_s, mv_s, mo_s, mg_s)
        phase_ml_scan(nc, S, mq_s, mk_s, mv_s, mg_s, tri_d, h_s)
        phase_ml_out(nc, S, h_s, mo_s, hgain, mem_out)
    return nc


def l1_inputs(inp, j):
    w_in = inp["w_in"][0]
    cols = []
    for seg in range(3):
        for g in range(3):
            s0 = seg * 3072 + g * 1024 + j * 128
            cols.append(np.arange(s0, s0 + 128))
    acols = np.concatenate(cols)
    base = 3 * 3072
    mcols = np.concatenate([np.arange(base + j * 256, base + (j + 1) * 256),
                            np.arange(base + 2048 + j * 256, base + 2048 + (j + 1) * 256),
                            np.arange(base + 4096 + j * 512, base + 4096 + (j + 1) * 512),
                            np.arange(base + 8192 + j * 512, base + 8192 + (j + 1) * 512),
                            np.array([base + 12288 + gi * 8 + j for gi in range(4)])])
    cs, masks = host_consts()
    gq = inp["attn_q_norm_gain"][0].reshape(-1)
    gk = inp["attn_k_norm_gain"][0].reshape(-1)
    f32 = np.float32
    return {
        "x": np.ascontiguousarray(inp["x"][0], dtype=f32),
        "w_att": np.ascontiguousarray(w_in[:, acols], dtype=f32),
        "w_ml": np.ascontiguousarray(w_in[:, mcols], dtype=f32),
        "g1t": np.ascontiguousarray(inp["norm1_gain"][0].reshape(32, 128).T, dtype=f32),
        "gqk": np.ascontiguousarray(np.broadcast_to(np.concatenate([gq, gk])[None, :], (128, 768)), dtype=f32),
        "cs": cs, "masks": masks,
        "gbias": np.ascontiguousarray(np.broadcast_to(inp["mlstm_gate_bias"][0][:, j][None, :], (128, 4)), dtype=f32),
        "hgain": np.ascontiguousarray(np.broadcast_to(inp["mlstm_head_norm_gain"][0][j][None, :], (128, 512)), dtype=f32),
        "tri_d": ml_consts(),
    }


def kernel(**inputs):
    inp = {k: np.asarray(v) for k, v in inputs.items()}
    f32 = np.float32
    cores = list(range(8))
    nc1 = build_l1()
    res = run_bass_kernel_spmd(nc1, [l1_inputs(inp, j) for j in cores], core_ids=cores)
    att = np.concatenate([np.asarray(r["att_out"]) for r in res.results], axis=1)
    mem = np.concatenate([np.asarray(r["mem_out"]) for r in res.results], axis=1)
    del res
    W2 = l2_weights(inp)
    x = np.ascontiguousarray(inp["x"][0], dtype=f32)
    maps = []
    for i in cores:
        sl = slice(i * TPC, (i + 1) * TPC)
        m = dict(W2)
        m.update({"x": np.ascontiguousarray(x[sl]), "att": np.ascontiguousarray(att[sl]), "mem": np.ascontiguousarray(mem[sl])})
        maps.append(m)
    nc2 = build_l2()
    res = run_bass_kernel_spmd(nc2, maps, core_ids=cores)
    x1 = np.concatenate([np.asarray(r["x1"]) for r in res.results], axis=0)
    del res, maps, W2
    nc3 = build_l3()
    res = run_bass_kernel_spmd(nc3, [l3_inputs(inp, j, x1) for j in cores], core_ids=cores)
    ys = [np.asarray(r["y"]) for r in res.results]
    del res
    maps = []
    for i in cores:
        sl = slice(i * TPC, (i + 1) * TPC)
        maps.append({"x1": np.ascontiguousarray(x1[sl]), "ys": np.ascontiguousarray(np.stack([y[sl] for y in ys], axis=0))})
    nc4 = build_l4()
    res = run_bass_kernel_spmd(nc4, maps, core_ids=cores)
    out = np.concatenate([np.asarray(r["out"]) for r in res.results], axis=0)
    return np.ascontiguousarray(out.reshape(1, SEQ, DM), dtype=f32)
```
